# Optimizing a Trainium2 kernel written in Bass

```python
import math
import jax, jax.numpy as jnp
from jax import lax
import numpy as np

D_MODEL = 2048
BATCH = 4
SEQ = 4096
DEPTH = 1

EPS = 1e-6
MLA_HEADS = 8
MLA_NOPE = 128
MLA_ROPE = 64
MLA_QK = MLA_NOPE + MLA_ROPE
MLA_V = 128
MLA_Q_RANK = 512
MLA_KV_RANK = 256
MLA_WIDTH = MLA_HEADS * MLA_V
ROPE_THETA = 10000.0
Q_BLOCK = 128
GLA_HEADS = 4
GLA_DK = 128
GLA_DV = 256
GLA_WIDTH = GLA_HEADS * GLA_DV
GLA_GATE_RANK = 16
GLA_TAU = 16.0
GLA_CHUNK = 64
PEER_HEADS = 8
PEER_NKEYS = 128
PEER_EXPERTS = PEER_NKEYS * PEER_NKEYS
PEER_QDIM = 256
PEER_HALF = PEER_QDIM // 2
PEER_TOPK = 16
PEER_TOKEN_BLOCK = 128
IN_SPLITS = (MLA_Q_RANK, MLA_KV_RANK, MLA_ROPE, GLA_HEADS * GLA_DK, GLA_HEADS * GLA_DK,
             GLA_WIDTH, GLA_GATE_RANK, GLA_WIDTH)
IN_WIDTH = MLA_Q_RANK + MLA_KV_RANK + MLA_ROPE + 2 * GLA_HEADS * GLA_DK + 2 * GLA_WIDTH + GLA_GATE_RANK
MIX_WIDTH = MLA_WIDTH + GLA_WIDTH

kernel_name = "hybrid_mla_gla_peer_adaln"


def rmsnorm(x, g):
    xf = x.astype(jnp.float32)
    y = xf * lax.rsqrt(jnp.mean(xf * xf, axis=-1, keepdims=True) + EPS)
    return (y * g.astype(jnp.float32)).astype(x.dtype)


def apply_rope(x, cos, sin):
    half = x.shape[-1] // 2
    x1, x2 = x[..., :half], x[..., half:]
    return jnp.concatenate([x1 * cos - x2 * sin, x2 * cos + x1 * sin], axis=-1)


def causal_block_attention(q, k, v):
    B, S, H, Dq = q.shape
    Dv = v.shape[-1]
    nb = S // Q_BLOCK
    qb = q.reshape(B, nb, Q_BLOCK, H, Dq).transpose(1, 0, 2, 3, 4)
    kpos = jnp.arange(S)
    scale = Dq ** -0.5

    def one_block(args):
        qi, bi = args
        s = jnp.einsum('bqhd,bkhd->bhqk', qi, k).astype(jnp.float32) * scale
        qpos = bi * Q_BLOCK + jnp.arange(Q_BLOCK)
        s = jnp.where(kpos[None, :] <= qpos[:, None], s, -jnp.inf)
        p = jax.nn.softmax(s, axis=-1).astype(v.dtype)
        return jnp.einsum('bhqk,bkhv->bqhv', p, v)

    out = lax.map(one_block, (qb, jnp.arange(nb)))
    return out.transpose(1, 0, 2, 3, 4).reshape(B, S, H, Dv)


def gla_chunked(q, k, v, log_a):
    B, S, H, dk = q.shape
    dv = v.shape[-1]
    C = GLA_CHUNK
    n = S // C

    def to_chunks(t):
        return t.reshape(B, n, C, H, t.shape[-1]).transpose(0, 3, 1, 2, 4)

    q, k, v, la = to_chunks(q), to_chunks(k), to_chunks(v), to_chunks(log_a)
    bcum = lax.cumsum(la, axis=3)
    b_last = bcum[:, :, :, -1:, :]
    b_mid = bcum[:, :, :, C // 2 - 1:C // 2, :]
    qe = q * jnp.exp(bcum - b_mid)
    ke = k * jnp.exp(b_mid - bcum)
    A = jnp.einsum('bhnid,bhnjd->bhnij', qe, ke)
    causal = jnp.tril(jnp.ones((C, C), dtype=bool))
    A = jnp.where(causal, A, 0.0)
    o_intra = jnp.einsum('bhnij,bhnjv->bhniv', A, v)
    U = jnp.einsum('bhnjd,bhnjv->bhndv', k * jnp.exp(b_last - bcum), v)
    decay = jnp.exp(b_last[:, :, :, 0, :])

    def step(state, inp):
        dec, u = inp
        return dec[..., None] * state + u, state

    S0 = jnp.zeros((B, H, dk, dv), q.dtype)
    _, S_before = lax.scan(step, S0, (decay.transpose(2, 0, 1, 3), U.transpose(2, 0, 1, 3, 4)))
    S_before = S_before.transpose(1, 2, 0, 3, 4)
    o_inter = jnp.einsum('bhnid,bhndv->bhniv', q * jnp.exp(bcum), S_before)
    o = o_intra + o_inter
    return o.transpose(0, 2, 3, 1, 4).reshape(B, S, H, dv)


def hybrid_mixer(h, cos, sin, w_in, q_norm_g, w_uq, kv_norm_g, w_ukv, mla_out_g,
                 gla_w_gate2, gla_b_gate, gla_out_g, w_out):
    B, S, _ = h.shape
    proj = h @ w_in
    offsets = [int(o) for o in np.cumsum(IN_SPLITS)[:-1]]
    cq, ckv, k_pe, gq, gk, gv, glr, gr = jnp.split(proj, offsets, axis=-1)

    q = (rmsnorm(cq, q_norm_g) @ w_uq).reshape(B, S, MLA_HEADS, MLA_QK)
    q_pe = apply_rope(q[..., MLA_NOPE:], cos[:, :, None, :], sin[:, :, None, :])
    q = jnp.concatenate([q[..., :MLA_NOPE], q_pe], axis=-1)
    kv = (rmsnorm(ckv, kv_norm_g) @ w_ukv).reshape(B, S, MLA_HEADS, MLA_NOPE + MLA_V)
    k_nope, v = kv[..., :MLA_NOPE], kv[..., MLA_NOPE:]
    k_pe = apply_rope(k_pe, cos, sin)
    k = jnp.concatenate([k_nope, jnp.broadcast_to(k_pe[:, :, None, :], (B, S, MLA_HEADS, MLA_ROPE))], axis=-1)
    o_mla = causal_block_attention(q, k, v)
    o_mla = rmsnorm(o_mla, mla_out_g.reshape(MLA_HEADS, MLA_V)).reshape(B, S, MLA_WIDTH)

    gq = gq.reshape(B, S, GLA_HEADS, GLA_DK).astype(jnp.float32) * (GLA_DK ** -0.5)
    gk = gk.reshape(B, S, GLA_HEADS, GLA_DK).astype(jnp.float32)
    gv = gv.reshape(B, S, GLA_HEADS, GLA_DV).astype(jnp.float32)
    log_a = jax.nn.log_sigmoid((glr @ gla_w_gate2 + gla_b_gate).astype(jnp.float32)) / GLA_TAU
    log_a = log_a.reshape(B, S, GLA_HEADS, GLA_DK)
    o_gla = gla_chunked(gq, gk, gv, log_a).astype(h.dtype)
    o_gla = rmsnorm(o_gla, gla_out_g.reshape(GLA_HEADS, GLA_DV)).reshape(B, S, GLA_WIDTH) * jax.nn.silu(gr)

    return jnp.concatenate([o_mla, o_gla], axis=-1) @ w_out


def peer_ffn(h, w_q, sub_keys, u_tab, v_tab):
    B, S, D = h.shape
    q = (h @ w_q).reshape(B, S, PEER_HEADS, 2, PEER_HALF)
    s = jnp.einsum('bshpd,hpkd->bshpk', q, sub_keys).astype(jnp.float32)
    top_s, top_i = lax.top_k(s, PEER_TOPK)
    cand = top_s[..., 0, :, None] + top_s[..., 1, None, :]
    cand = cand.reshape(B, S, PEER_HEADS, PEER_TOPK * PEER_TOPK)
    best_s, best_c = lax.top_k(cand, PEER_TOPK)
    i1 = jnp.take_along_axis(top_i[..., 0, :], best_c // PEER_TOPK, axis=-1)
    i2 = jnp.take_along_axis(top_i[..., 1, :], best_c % PEER_TOPK, axis=-1)
    expert = i1 * PEER_NKEYS + i2
    gate = jax.nn.softmax(best_s, axis=-1).astype(h.dtype)
    E = PEER_HEADS * PEER_TOPK
    nb = (B * S) // PEER_TOKEN_BLOCK
    xb = h.reshape(nb, PEER_TOKEN_BLOCK, D)
    eb = expert.reshape(nb, PEER_TOKEN_BLOCK, E)
    gb = gate.reshape(nb, PEER_TOKEN_BLOCK, E)

    def block(args):
        xt, et, gt = args
        u = jnp.take(u_tab, et, axis=0)
        act = jax.nn.gelu(jnp.einsum('td,ted->te', xt, u), approximate=False)
        return jnp.einsum('te,ted->td', gt * act, jnp.take(v_tab, et, axis=0))

    out = lax.map(block, (xb, eb, gb))
    return out.reshape(B, S, D)


def setup_inputs(seed: int = 0) -> dict:
    key = jax.random.key(seed)
    ks = jax.random.split(key, 24)
    L, D = DEPTH, D_MODEL
    f32 = jnp.float32

    def nrm(k, shape, scale):
        return jax.random.normal(k, shape, f32) * scale

    def gain(k, shape):
        return 1.0 + 0.02 * jax.random.normal(k, shape, f32)

    return {
        "x": nrm(ks[0], (BATCH, SEQ, D), 1.0),
        "c": nrm(ks[1], (BATCH, D), 1.0),
        "positions": jnp.broadcast_to(jnp.arange(SEQ, dtype=jnp.int32)[None, :], (BATCH, SEQ)),
        "ada_w": nrm(ks[2], (L, D, 6 * D), 0.5 * D ** -0.5),
        "ada_b": nrm(ks[3], (L, 6 * D), 0.02),
        "mix_norm_g": gain(ks[4], (L, D)),
        "w_in": nrm(ks[5], (L, D, IN_WIDTH), D ** -0.5),
        "mla_q_norm_g": gain(ks[6], (L, MLA_Q_RANK)),
        "mla_w_uq": nrm(ks[7], (L, MLA_Q_RANK, MLA_HEADS * MLA_QK), MLA_Q_RANK ** -0.5),
        "mla_kv_norm_g": gain(ks[8], (L, MLA_KV_RANK)),
        "mla_w_ukv": nrm(ks[9], (L, MLA_KV_RANK, MLA_HEADS * (MLA_NOPE + MLA_V)), MLA_KV_RANK ** -0.5),
        "mla_out_norm_g": gain(ks[10], (L, MLA_WIDTH)),
        "gla_w_gate2": nrm(ks[11], (L, GLA_GATE_RANK, GLA_HEADS * GLA_DK), GLA_GATE_RANK ** -0.5),
        "gla_b_gate": nrm(ks[12], (L, GLA_HEADS * GLA_DK), 0.1),
        "gla_out_norm_g": gain(ks[13], (L, GLA_WIDTH)),
        "w_out": nrm(ks[14], (L, MIX_WIDTH, D), MIX_WIDTH ** -0.5),
        "ffn_norm_g": gain(ks[15], (L, D)),
        "peer_w_q": nrm(ks[16], (L, D, PEER_HEADS * PEER_QDIM), D ** -0.5),
        "peer_sub_keys": nrm(ks[17], (L, PEER_HEADS, 2, PEER_NKEYS, PEER_HALF), PEER_HALF ** -0.5),
        "peer_u": nrm(ks[18], (L, PEER_EXPERTS, D), D ** -0.5),
        "peer_v": nrm(ks[19], (L, PEER_EXPERTS, D), 1.0),
        "final_norm_g": gain(ks[20], (D,)),
    }


def reference(x, c, positions, ada_w, ada_b, mix_norm_g, w_in, mla_q_norm_g, mla_w_uq,
              mla_kv_norm_g, mla_w_ukv, mla_out_norm_g, gla_w_gate2, gla_b_gate,
              gla_out_norm_g, w_out, ffn_norm_g, peer_w_q, peer_sub_keys, peer_u, peer_v,
              final_norm_g):
    inv_freq = ROPE_THETA ** (-jnp.arange(0, MLA_ROPE, 2, dtype=jnp.float32) / MLA_ROPE)
    ang = positions.astype(jnp.float32)[..., None] * inv_freq
    cos = jnp.cos(ang).astype(x.dtype)
    sin = jnp.sin(ang).astype(x.dtype)
    c_act = jax.nn.silu(c)
    for l in range(DEPTH):
        mod = (c_act @ ada_w[l] + ada_b[l])[:, None, :]
        sh1, sc1, g1, sh2, sc2, g2 = jnp.split(mod, 6, axis=-1)
        h = rmsnorm(x, mix_norm_g[l]) * (1.0 + sc1) + sh1
        x = x + g1 * hybrid_mixer(h, cos, sin, w_in[l], mla_q_norm_g[l], mla_w_uq[l],
                                  mla_kv_norm_g[l], mla_w_ukv[l], mla_out_norm_g[l],
                                  gla_w_gate2[l], gla_b_gate[l], gla_out_norm_g[l], w_out[l])
        h = rmsnorm(x, ffn_norm_g[l]) * (1.0 + sc2) + sh2
        x = x + g2 * peer_ffn(h, peer_w_q[l], peer_sub_keys[l], peer_u[l], peer_v[l])
    return rmsnorm(x, final_norm_g)
```

```python
import numpy as np
import concourse.bass as bass
import concourse.mybir as mybir
from concourse.bass_utils import run_bass_kernel_spmd
from contextlib import ExitStack

F32 = mybir.dt.float32
BF16 = mybir.dt.bfloat16
I32 = mybir.dt.int32
ALU = mybir.AluOpType
AF = mybir.ActivationFunctionType
AX = mybir.AxisListType

D = 2048
NT = 32
NOWN = 16
INW = 3920
EPS = 1e-6
O_CQ, O_CKV, O_KPE, O_GQ, O_GK, O_GV, O_GLR, O_GR = 0, 512, 768, 832, 1344, 1856, 2880, 2896
C_C, C_G1, C_G2, C_QG, C_KVG, C_MOG, C_GOG, C_INVF, C_SGN, C_FLAG, C_NEG, C_ONE = 0, 16, 32, 48, 52, 54, 62, 70, 71, 72, 73, 74
NVEC = 80


DBG_TILES = 99
DBG_SUB = 99


class _StopDbg(Exception):
    pass


def _sub(n):
    if DBG_SUB <= n:
        raise _StopDbg()


class Buf:
    __slots__ = ("w", "r", "excl")

    def __init__(self, excl=False):
        self.w = None
        self.r = []
        self.excl = excl


class _Grp:
    def __init__(self, s, eng, reads, writes):
        self.s, self.eng, self.reads, self.writes = s, eng, reads, writes
        self.last = None

    def __enter__(self):
        self.s._waits(self.eng, self.reads, self.writes)
        return self

    def __exit__(self, *a):
        if a[0] is not None:
            return False
        s = self.s
        self.last.then_inc(s.sem[self.eng], 1)
        s.cnt[self.eng] += 1
        s._register((self.eng, s.cnt[self.eng]), self.reads, self.writes)
        return False


class Sched:
    def __init__(self, nc, es, n_dma_sems=32):
        self.nc = nc
        self.engs = {"pe": nc.tensor, "act": nc.scalar, "dve": nc.vector, "pool": nc.gpsimd, "sp": nc.sync}
        self.sem = {k: es.enter_context(nc.semaphore("s_" + k)) for k in self.engs}
        self.cnt = {k: 0 for k in self.engs}
        self.dsem = [es.enter_context(nc.semaphore("d_%d" % i)) for i in range(n_dma_sems)]
        self.dcnt = [0] * n_dma_sems
        self.dnext = 0
        self.dnext_sw = 0
        self.seen = {k: {} for k in self.engs}

    def _h(self, key):
        return self.sem[key] if isinstance(key, str) else self.dsem[key[1]]

    def _wait(self, eng, ev):
        key, val = ev
        if self.seen[eng].get(key, 0) >= val:
            return
        if key == eng and eng in ("pe", "sp"):
            return
        self.engs[eng].wait_ge(self._h(key), val)
        self.seen[eng][key] = val

    def _waits(self, eng, reads, writes):
        evs = {}
        for b in reads:
            if b.w is not None:
                evs[b.w[0]] = max(evs.get(b.w[0], 0), b.w[1])
        for b in writes:
            if b.w is not None:
                evs[b.w[0]] = max(evs.get(b.w[0], 0), b.w[1])
            for e in b.r:
                evs[e[0]] = max(evs.get(e[0], 0), e[1])
        for k, v in evs.items():
            self._wait(eng, (k, v))

    def _register(self, ev, reads, writes):
        for b in writes:
            b.w = ev
            b.r = []
        for b in reads:
            if b in writes:
                continue
            b.r = [e for e in b.r if e[0] != ev[0]] + [ev]

    def grp(self, eng, reads=(), writes=()):
        reads, writes = list(reads), list(writes)
        for b in reads:
            if b.excl and b not in writes:
                writes.append(b)
        return _Grp(self, eng, reads, writes)

    def op(self, eng, fn, reads=(), writes=()):
        with self.grp(eng, reads, writes) as g:
            g.last = fn()

    def dma(self, q, out, in_, reads=(), writes=(), **kw):
        reads, writes = list(reads), list(writes)
        self._waits(q, reads, writes)
        nsw = 8
        if q == "pool":
            s = self.dnext_sw
            self.dnext_sw = (s + 1) % nsw
        else:
            s = nsw + self.dnext
            self.dnext = (self.dnext + 1) % (len(self.dsem) - nsw)
        if self.dcnt[s] > 0:
            self._wait(q, (("d", s), 16 * self.dcnt[s]))
        inst = self.engs[q].dma_start(out=out, in_=in_, **kw)
        inst.then_inc(self.dsem[s], 16)
        self.dcnt[s] += 1
        self._register((("d", s), 16 * self.dcnt[s]), reads, writes)

    def barrier(self):
        for e in self.engs:
            for k in self.engs:
                if k != e and k != "sp" and self.cnt[k] > 0:
                    self._wait(e, (k, self.cnt[k]))
            for i in range(len(self.dsem)):
                if self.dcnt[i] > 0:
                    self._wait(e, (("d", i), 16 * self.dcnt[i]))

    def finish(self, eng, bufs):
        for b in bufs:
            if b.w is not None:
                self._wait(eng, b.w)


class T:
    def __init__(self, t):
        self.t = t
        self.b = Buf()
        self.b2 = Buf()


def build_program(stop=99, debug=False):
    nc = bass.Bass("TRN2", target_bir_lowering=False)
    dram_in = lambda n, sh, dt=F32: nc.dram_tensor(n, sh, dt, kind="ExternalInput").ap()
    xs = dram_in("xs", [NT * 128, D])
    pos = dram_in("pos", [1, NT * 128], I32)
    vec = dram_in("vec", [128, NVEC])
    cmat = dram_in("cmat", [128, 5, 128])
    ada_w = dram_in("ada_w", [D, 6 * D])
    ada_b = dram_in("ada_b", [1, 6 * D])
    w_in = dram_in("w_in", [D, INW])
    w_uq = dram_in("w_uq", [512, 1536])
    w_ukv = dram_in("w_ukv", [256, 2048])
    w_g2 = dram_in("w_g2", [16, 512])
    b_g = dram_in("b_g", [1, 512])
    w_out = dram_in("w_out", [D, D])
    w_pq = dram_in("w_pq", [D, D])
    subk = dram_in("subk", [16, 128, 128])
    u_tab = dram_in("u_tab", [16384, D])
    v_tab = dram_in("v_tab", [16384, D])
    fin_g = dram_in("fin_g", [1, D])
    y_out = nc.dram_tensor("y_out", [NOWN * 128, D], F32, kind="ExternalOutput").ap()

    scr = lambda n, sh, dt: (nc.dram_tensor(n, sh, dt, kind="ExternalOutput").ap() if debug else nc.dram_tensor(n, sh, dt).ap())
    ROPE = scr("rope_s", [64, 2, NT * 128], F32)
    KN = scr("kn_s", [NT, 128, 8, 128], BF16)
    KR = scr("kr_s", [NT, 64, 128], BF16)
    VS = scr("v_s", [NT, 128, 8, 128], BF16)
    QN = scr("qn_s", [NOWN, 128, 8, 128], BF16)
    QR = scr("qr_s", [NOWN, 64, 8, 128], BF16)
    MIX = scr("mix_s", [NOWN, 128, 16, 128], BF16)
    X2 = scr("x2_s", [NOWN, 128, D], F32)
    H2T = scr("h2t_s", [NOWN, 128, 16, 128], BF16)
    SALL = scr("sall_s", [NOWN, 128, 16, 128], F32)
    TK = scr("tk_s", [NOWN, 128, 16], F32)
    WT = scr("wt_s", [NOWN, 128, 128, 128], BF16)
    G12 = scr("g12_s", [2, 128, D], F32)
    HT1 = scr("ht1_s", [NOWN, 128, 16, 128], BF16)
    bHT1 = [Buf() for _ in range(NOWN)]
    bROPE, bG12 = Buf(), Buf()
    bKN = [Buf() for _ in range(NT)]
    bKR = [Buf() for _ in range(NT)]
    bVS = [Buf() for _ in range(NT)]
    bQ = [Buf() for _ in range(NOWN)]
    bMIXm = [Buf() for _ in range(NOWN)]
    bMIXg = [Buf() for _ in range(NOWN)]
    bX2 = [Buf() for _ in range(NOWN)]
    bH2T = [Buf() for _ in range(NOWN)]
    bSALL = [Buf() for _ in range(NOWN)]
    bY = [Buf() for _ in range(NOWN)]

    with ExitStack() as es:
        s = Sched(nc, es)

        def sbt(st, name, shape, dt):
            return T(st.enter_context(nc.sbuf_tensor(name, shape, dt)))

        def pst(st, name, shape, dt=F32):
            t_ = T(st.enter_context(nc.psum_tensor(name, shape, dt)))
            t_.b.excl = True
            return t_

        V, A, P = nc.vector, nc.scalar, nc.gpsimd
        fl = lambda T_: T_.t[:].rearrange("p a b -> p (a b)")
        skT = sbt(es, "skT", [128, 16, 128], BF16)

        vecs = sbt(es, "vecs", [128, NVEC], F32)
        cm = sbt(es, "cm", [128, 5, 128], F32)
        identb = sbt(es, "identb", [128, 128], BF16)
        trib = sbt(es, "trib", [128, 128], BF16)
        onesb = sbt(es, "onesb", [128, 128], BF16)
        onesf = sbt(es, "onesf", [128, 128], F32)
        AB = sbt(es, "AB", [128, 64], F32)
        epsT = sbt(es, "epsT", [128, 1], F32)
        s.dma("sp", vecs.t[:], vec, writes=[vecs.b])
        s.dma("sp", cm.t[:], cmat, writes=[cm.b])
        s.op("dve", lambda: V.tensor_copy(out=identb.t[:], in_=cm.t[:, 0, :]), [cm.b], [identb.b])
        s.op("dve", lambda: V.tensor_copy(out=trib.t[:], in_=cm.t[:, 1, :]), [cm.b], [trib.b])
        s.op("dve", lambda: V.memset(onesb.t[:], 1.0), [], [onesb.b])
        s.op("dve", lambda: V.memset(onesf.t[:], 1.0), [], [onesf.b])
        s.op("dve", lambda: V.memset(epsT.t[:], EPS), [], [epsT.b])

        def rstd_from(st_eng_dst, src_ap, n, reads, dstT, shape_ap):
            s.op("dve", lambda: V.tensor_scalar(out=shape_ap, in0=src_ap, scalar1=1.0 / n, scalar2=EPS,
                                                op0=ALU.mult, op1=ALU.add), reads, [dstT.b])
            s.op("act", lambda: A.activation(out=shape_ap, in_=shape_ap, func=AF.Sqrt), [dstT.b], [dstT.b])
            s.op("dve", lambda: V.reciprocal(out=shape_ap, in_=shape_ap), [dstT.b], [dstT.b])

        s.barrier()
        with ExitStack() as p0:
            pi = sbt(p0, "r_pi", [64, 1024], I32)
            pf = sbt(p0, "r_pf", [64, 1024], F32)
            pk = sbt(p0, "r_pk", [64, 1024], F32)
            pk2 = sbt(p0, "r_pk2", [64, 1024], F32)
            rout = sbt(p0, "r_out", [64, 2, 1024], F32)
            for ch in range(NT * 128 // 1024):
                sl = slice(ch * 1024, (ch + 1) * 1024)
                s.dma("sp", pi.t[:], pos[:, sl].to_broadcast([64, 1024]), writes=[pi.b])
                s.op("dve", lambda: V.tensor_copy(out=pf.t[:], in_=pi.t[:]), [pi.b], [pf.b])
                s.op("dve", lambda: V.tensor_scalar(out=pf.t[:], in0=pf.t[:], scalar1=vecs.t[0:64, C_INVF:C_INVF + 1],
                                                    scalar2=None, op0=ALU.mult), [pf.b, vecs.b], [pf.b])
                for which in range(2):
                    src = pf
                    if which == 0:
                        s.op("dve", lambda: V.tensor_scalar(out=pk2.t[:], in0=pf.t[:], scalar1=0.25, scalar2=None,
                                                            op0=ALU.add), [pf.b], [pk2.b])
                        src = pk2
                    else:
                        s.op("dve", lambda: V.tensor_copy(out=pk2.t[:], in_=pf.t[:]), [pf.b], [pk2.b])
                        src = pk2
                    s.op("dve", lambda: V.tensor_copy(out=pi.t[:], in_=src.t[:]), [src.b], [pi.b])
                    s.op("dve", lambda: V.tensor_copy(out=pk.t[:], in_=pi.t[:]), [pi.b], [pk.b])
                    s.op("dve", lambda: V.tensor_tensor(out=src.t[:], in0=src.t[:], in1=pk.t[:], op=ALU.subtract),
                         [src.b, pk.b], [src.b])
                    s.op("dve", lambda: V.tensor_scalar(out=pk.t[:], in0=src.t[:], scalar1=0.5, scalar2=None,
                                                        op0=ALU.is_ge), [src.b], [pk.b])
                    s.op("dve", lambda: V.tensor_tensor(out=src.t[:], in0=src.t[:], in1=pk.t[:], op=ALU.subtract),
                         [src.b, pk.b], [src.b])
                    s.op("dve", lambda: V.tensor_scalar(out=pk.t[:], in0=src.t[:], scalar1=-0.5, scalar2=None,
                                                        op0=ALU.is_lt), [src.b], [pk.b])
                    s.op("dve", lambda: V.tensor_tensor(out=src.t[:], in0=src.t[:], in1=pk.t[:], op=ALU.add),
                         [src.b, pk.b], [src.b])
                    s.op("act", lambda: A.activation(out=rout.t[:, which, :], in_=src.t[:], func=AF.Sin,
                                                     scale=float(2 * np.pi)), [src.b], [rout.b])
                s.op("dve", lambda: V.tensor_scalar(out=rout.t[:, 1, :], in0=rout.t[:, 1, :],
                                                    scalar1=vecs.t[0:64, C_SGN:C_SGN + 1], scalar2=None, op0=ALU.mult),
                     [rout.b, vecs.b], [rout.b])
                s.dma("sp", ROPE[:, :, sl], rout.t[:], reads=[rout.b], writes=[bROPE])

        s.barrier()
        with ExitStack() as p0:
            cact = sbt(p0, "cact", [128, 16], F32)
            adab = sbt(p0, "adab", [1, 6 * D], F32)
            modrow = sbt(p0, "modrow", [1, 6 * D], F32)
            wblk = [sbt(p0, "wblk%d" % i, [128, 16, 512], F32) for i in range(2)]
            pm = [pst(p0, "pm%d" % i, [128, 512]) for i in range(2)]
            pcol = pst(p0, "pcol", [128, 64])
            cols = sbt(p0, "cols", [128, 64], F32)
            gb = sbt(p0, "gb", [128, D], F32)
            s.dma("sp", adab.t[:], ada_b, writes=[adab.b])
            s.op("act", lambda: A.activation(out=cact.t[:], in_=vecs.t[:, C_C:C_C + 16], func=AF.Silu),
                 [vecs.b], [cact.b])
            adv = ada_w.rearrange("(k p) n -> p k n", p=128)
            for blk in range(24):
                wb = wblk[blk % 2]
                pp = pm[blk % 2]
                s.dma("sp", wb.t[:], adv[:, :, blk * 512:(blk + 1) * 512], writes=[wb.b])
                with s.grp("pe", [wb.b, cact.b], [pp.b]) as g:
                    for k in range(16):
                        g.last = nc.tensor.matmul(pp.t[0:1, :], lhsT=cact.t[:, k:k + 1], rhs=wb.t[:, k, :],
                                                  start=(k == 0), stop=(k == 15))
                s.op("dve", lambda: V.tensor_tensor(out=modrow.t[0:1, blk * 512:(blk + 1) * 512], in0=pp.t[0:1, :],
                                                    in1=adab.t[0:1, blk * 512:(blk + 1) * 512], op=ALU.add),
                     [pp.b, adab.b], [modrow.b])
            with s.grp("pe", [modrow.b, onesf.b], [pcol.b]) as g:
                for vi, v in enumerate((0, 1, 3, 4)):
                    for k in range(16):
                        off = v * D + k * 128
                        g.last = nc.tensor.matmul(pcol.t[:, vi * 16 + k:vi * 16 + k + 1],
                                                  lhsT=modrow.t[0:1, off:off + 128], rhs=onesf.t[0:1, 0:1],
                                                  start=True, stop=True)
            s.op("dve", lambda: V.tensor_copy(out=cols.t[:], in_=pcol.t[:]), [pcol.b], [cols.b])
            for (dst, gcol, sc, sh) in ((0, C_G1, 16, 0), (32, C_G2, 48, 32)):
                s.op("dve", lambda: V.scalar_tensor_tensor(out=AB.t[:, dst:dst + 16], in0=cols.t[:, sc:sc + 16], scalar=1.0,
                                                           in1=vecs.t[:, gcol:gcol + 16], op0=ALU.add, op1=ALU.mult),
                     [cols.b, vecs.b], [AB.b])
                s.op("dve", lambda: V.tensor_copy(out=AB.t[:, dst + 16:dst + 32], in_=cols.t[:, sh:sh + 16]),
                     [cols.b], [AB.b])
            for gi, v in enumerate((2, 5)):
                for j in range(4):
                    pp = pm[j % 2]
                    s.op("pe", lambda: nc.tensor.matmul(pp.t[:], lhsT=onesf.t[0:1, :],
                                                        rhs=modrow.t[0:1, v * D + j * 512:v * D + (j + 1) * 512],
                                                        start=True, stop=True), [modrow.b, onesf.b], [pp.b])
                    s.op("dve", lambda: V.tensor_copy(out=gb.t[:, j * 512:(j + 1) * 512], in_=pp.t[:]), [pp.b], [gb.b])
                s.dma("sp", G12[gi], gb.t[:], reads=[gb.b], writes=[bG12])

        s.barrier()
        with ExitStack() as p1:
            win = sbt(p1, "win", [128, 16, O_GR], BF16)
            kpesw = sbt(p1, "kpesw", [128, 16, 64], BF16)
            wuq = sbt(p1, "wuq", [128, 4, 1536], BF16)
            wqsw = sbt(p1, "wqsw", [128, 4, 8, 64], BF16)
            wukv = sbt(p1, "wukv", [128, 2, 2048], BF16)
            w2f = sbt(p1, "w2f", [33, 512], F32)
            w2a = sbt(p1, "w2a", [33, 512], BF16)
            glrT = sbt(p1, "glrT", [33, 128], BF16)
            Sst = sbt(p1, "Sst", [128, 4, 256], F32)
            Sbf = sbt(p1, "Sbf", [128, 4, 256], BF16)
            w_in_v = w_in.rearrange("(k p) n -> p k n", p=128)
            winb = {}
            for nm_, a_, b_ in (("ckv", O_CKV, O_GQ), ("glr", O_GLR, O_GR), ("gk", O_GK, O_GV), ("gv", O_GV, O_GLR),
                                ("cq", O_CQ, O_CKV), ("gq", O_GQ, O_GK)):
                winb[nm_] = Buf()
                step_ = 512
                for c0_ in range(a_, b_, step_):
                    c1_ = min(b_, c0_ + step_)
                    s.dma("pool", win.t[:, :, c0_:c1_], w_in_v[:, :, c0_:c1_], writes=[winb[nm_]])
            s.dma("pool", wuq.t[:], w_uq.rearrange("(k p) n -> p k n", p=128), writes=[wuq.b])
            s.dma("pool", wukv.t[:], w_ukv.rearrange("(k p) n -> p k n", p=128), writes=[wukv.b])
            s.op("dve", lambda: V.memset(w2f.t[:], 0.0), [], [w2f.b])
            s.dma("sp", w2f.t[0:16, :], w_g2, writes=[w2f.b])
            s.dma("sp", w2f.t[32:33, :], b_g, writes=[w2f.b])
            s.op("dve", lambda: V.tensor_copy(out=w2a.t[:], in_=w2f.t[:]), [w2f.b], [w2a.b])
            s.op("dve", lambda: V.memset(glrT.t[:], 0.0), [], [glrT.b])
            s.op("dve", lambda: V.memset(glrT.t[32:33, :], 1.0), [glrT.b], [glrT.b])
            s.op("dve", lambda: V.memset(Sst.t[:], 0.0), [], [Sst.b])
            s.op("dve", lambda: V.memset(Sbf.t[:], 0.0), [], [Sbf.b])
            for k in range(4):
                s.op("dve", lambda: V.tensor_scalar(out=wuq.t[:, k, :], in0=wuq.t[:, k, :],
                                                    scalar1=vecs.t[:, C_QG + k:C_QG + k + 1], scalar2=None, op0=ALU.mult),
                     [wuq.b, vecs.b], [wuq.b])
            for k in range(2):
                s.op("dve", lambda: V.tensor_scalar(out=wukv.t[:, k, :], in0=wukv.t[:, k, :],
                                                    scalar1=vecs.t[:, C_KVG + k:C_KVG + k + 1], scalar2=None, op0=ALU.mult),
                     [wukv.b, vecs.b], [wukv.b])
            wuq4 = wuq.t[:].rearrange("p k (h c) -> p k h c", c=192)
            s.op("dve", lambda: V.tensor_copy(out=wqsw.t[:, :, :, 0:32], in_=wuq4[:, :, :, 160:192]), [wuq.b], [wqsw.b])
            s.op("dve", lambda: V.tensor_copy(out=wqsw.t[:, :, :, 32:64], in_=wuq4[:, :, :, 128:160]), [wqsw.b, wuq.b], [wqsw.b])
            s.op("dve", lambda: V.tensor_copy(out=kpesw.t[:, :, 0:32], in_=win.t[:, :, O_KPE + 32:O_KPE + 64]), [winb["ckv"]], [kpesw.b])
            s.op("dve", lambda: V.tensor_copy(out=kpesw.t[:, :, 32:64], in_=win.t[:, :, O_KPE:O_KPE + 32]), [kpesw.b, winb["ckv"]], [kpesw.b])

            xt = [sbt(p1, "xt%d" % i, [128, D], F32) for i in range(1)]
            rt = [sbt(p1, "rt%d" % i, [64, 2, 128], F32) for i in range(3)]
            xns = [sbt(p1, "xn%d" % i, [128, D], BF16) for i in range(2)]
            hTs = [sbt(p1, "hT%d" % i, [128, 16, 128], BF16) for i in range(2)]
            hT = hTs[0]
            st1s = [sbt(p1, "st1%d" % i, [128, 4], F32) for i in range(2)]
            latr = sbt(p1, "latr", [128, 4, 128], F32)
            latsq = sbt(p1, "latsq", [128, 4, 128], BF16)
            latn = sbt(p1, "latn", [128, 4, 128], BF16)
            rsb = sbt(p1, "rsb", [128, 4, 128], F32)
            kNs = sbt(p1, "kNs", [128, 8, 128], BF16)
            vs = sbt(p1, "vs", [128, 8, 128], BF16)
            rp1 = sbt(p1, "rp1", [64, 4, 128], F32)
            rp2 = sbt(p1, "rp2", [64, 4, 128], F32)
            kRs = sbt(p1, "kRs", [64, 128], BF16)
            qNs = sbt(p1, "qNs", [128, 8, 128], BF16)
            qRs = sbt(p1, "qRs", [64, 8, 128], BF16)
            la = sbt(p1, "la", [128, 512], F32)
            gktm = sbt(p1, "gktm", [128, 512], F32)
            gvb = sbt(p1, "gvb", [128, 1024], BF16)
            e1 = sbt(p1, "e1", [128, 4, 128], BF16)
            e2 = sbt(p1, "e2", [128, 4, 128], BF16)
            e4 = sbt(p1, "e4", [128, 4, 128], BF16)
            e3 = sbt(p1, "e3", [128, 512], BF16)
            dec = sbt(p1, "dec", [128, 4], F32)
            qeT = sbt(p1, "qeT", [128, 4, 128], BF16)
            qbT = sbt(p1, "qbT", [128, 4, 128], BF16)
            keT = sbt(p1, "keT", [128, 4, 128], BF16)
            ke2 = sbt(p1, "ke2", [128, 512], BF16)
            ATm = sbt(p1, "ATm", [128, 4, 128], BF16)
            ogr = sbt(p1, "ogr", [128, 8, 128], F32)
            ogsq = sbt(p1, "ogsq", [128, 8, 128], BF16)
            mixg = sbt(p1, "mixg", [128, 8, 128], BF16)
            PT = pst(p1, "PT", [128, 16, 128], BF16)
            PB = [pst(p1, "PB%d" % i, [128, 4, 128]) for i in range(6)]
            pbi = [0]

            def bank():
                b = PB[pbi[0] % 6]
                pbi[0] += 1
                return b

            def load_tile(i):
                s.dma("sp", xt[0].t[:], xs[i * 128:(i + 1) * 128, :], writes=[xt[0].b])
                s.dma("sp", rt[i % 3].t[:], ROPE[:, :, i * 128:(i + 1) * 128], reads=[bROPE], writes=[rt[i % 3].b])

            def norm_front(i_):
                xtile, xn, st1 = xt[0], xns[i_ % 2], st1s[i_ % 2]
                s.op("act", lambda: A.activation(out=xn.t[:], in_=xtile.t[:], func=AF.Square, accum_out=st1.t[:, 0:1]),
                     [xtile.b], [xn.b, st1.b])
                rstd_from(None, st1.t[:, 0:1], D, [st1.b], st1, st1.t[:, 1:2])
                s.op("act", lambda: A.activation(out=xn.t[:], in_=xtile.t[:], func=AF.Copy, scale=st1.t[:, 1:2]),
                     [xtile.b, st1.b], [xn.b])
                if i_ + 1 < NT:
                    load_tile(i_ + 1)

            def norm_back(i_, abcol, hdst):
                xn = xns[i_ % 2]
                with s.grp("pe", [xn.b, identb.b], [PT.b]) as g:
                    for k in range(16):
                        g.last = nc.tensor.transpose(PT.t[:, k, :], xn.t[:, k * 128:(k + 1) * 128], identb.t[:])
                for k in range(16):
                    if k % 2 == 0:
                        s.op("dve", lambda: V.tensor_scalar(out=hdst.t[:, k, :], in0=PT.t[:, k, :],
                                                            scalar1=AB.t[:, abcol + k:abcol + k + 1],
                                                            scalar2=AB.t[:, abcol + 16 + k:abcol + 17 + k],
                                                            op0=ALU.mult, op1=ALU.add), [PT.b, AB.b], [hdst.b])
                    else:
                        s.op("act", lambda: A.activation(out=hdst.t[:, k, :], in_=PT.t[:, k, :], func=AF.Identity,
                                                         scale=AB.t[:, abcol + k:abcol + k + 1],
                                                         bias=AB.t[:, abcol + 16 + k:abcol + 17 + k]), [PT.b, AB.b], [hdst.b2])

            def proj_fm(pb, slot, col0, m, rows=None, wt=None):
                wt = wt if wt is not None else win
                for k in range(16):
                    lhsT = wt.t[:, k, col0:col0 + m]
                    last = nc.tensor.matmul(pb.t[0:m, slot, :], lhsT=lhsT, rhs=hT.t[:, k, :], start=(k == 0), stop=(k == 15))
                return last

            def latent_norm(pb, nch, n):
                s.op("act", lambda: A.activation(out=latsq.t[:, 0:nch, :], in_=pb.t[:, 0:nch, :], func=AF.Square), [pb.b], [latsq.b])
                s.op("dve", lambda: V.tensor_copy(out=latr.t[:, 0:nch, :], in_=pb.t[:, 0:nch, :]), [pb.b], [latr.b])
                _sub(2.2)
                pr = bank()
                with s.grp("pe", [latsq.b, onesb.b], [pr.b]) as g:
                    for c in range(nch):
                        g.last = nc.tensor.matmul(pr.t[:, 0, :], lhsT=onesb.t[:], rhs=latsq.t[:, c, :], start=(c == 0), stop=(c == nch - 1))
                _sub(2.4)
                rstd_from(None, pr.t[:, 0, :], n, [pr.b], rsb, rsb.t[:, 0, :])
                _sub(2.6)
                s.op("dve", lambda: V.tensor_tensor(out=latn.t[:, 0:nch, :], in0=latr.t[:, 0:nch, :],
                                                    in1=rsb.t[:, 0:1, :].to_broadcast([128, nch, 128]), op=ALU.mult),
                     [latr.b, rsb.b], [latn.b])

            load_tile(0)
            try:
                for i in range(min(NT if stop >= 1 else 0, DBG_TILES)):
                    own = (i % 2 == 1)
                    j = i // 2
                    r_i = rt[i % 3]
                    hT = hTs[i % 2]
                    if i == 0:
                        norm_front(0)
                        norm_back(0, 0, hT)
                    _sub(1)
                    if own:
                        s.dma("sp", HT1[j], hT.t[:], reads=[hT.b, hT.b2], writes=[bHT1[j]])
                    pb = bank()
                    with s.grp("pe", [hT.b, hT.b2, winb["ckv"], kpesw.b], [pb.b]) as g:
                        proj_fm(pb, 0, O_CKV, 128)
                        proj_fm(pb, 1, O_CKV + 128, 128)
                        proj_fm(pb, 2, O_KPE, 64)
                        g.last = proj_fm(pb, 3, 0, 64, wt=kpesw)
                    s.op("dve", lambda: V.tensor_tensor(out=rp1.t[:, 0, :], in0=pb.t[0:64, 2, :], in1=r_i.t[:, 0, :], op=ALU.mult),
                         [pb.b, r_i.b], [rp1.b])
                    s.op("dve", lambda: V.tensor_tensor(out=rp2.t[:, 0, :], in0=pb.t[0:64, 3, :], in1=r_i.t[:, 1, :], op=ALU.mult),
                         [pb.b, r_i.b], [rp2.b])
                    s.op("pool", lambda: P.tensor_tensor(out=kRs.t[:], in0=rp1.t[:, 0, :], in1=rp2.t[:, 0, :], op=ALU.add),
                         [rp1.b, rp2.b], [kRs.b])
                    s.dma("sp", KR[i], kRs.t[:], reads=[kRs.b], writes=[bKR[i]])
                    _sub(2)
                    latent_norm(pb, 2, 256)
                    _sub(3)
                    pk0, pk1 = bank(), bank()
                    with s.grp("pe", [latn.b, wukv.b], [pk0.b, pk1.b]) as g:
                        for h in range(8):
                            pk = pk0 if h < 4 else pk1
                            for c in range(2):
                                g.last = nc.tensor.matmul(pk.t[:, h % 4, :], lhsT=wukv.t[:, c, h * 256:h * 256 + 128],
                                                          rhs=latn.t[:, c, :], start=(c == 0), stop=(c == 1))
                    s.op("act", lambda: A.copy(out=kNs.t[:, 0:4, :], in_=pk0.t[:]), [pk0.b], [kNs.b])
                    s.op("act", lambda: A.copy(out=kNs.t[:, 4:8, :], in_=pk1.t[:]), [pk1.b, kNs.b], [kNs.b])
                    s.dma("sp", KN[i], kNs.t[:], reads=[kNs.b], writes=[bKN[i]])
                    _sub(4)
                    pv0, pv1 = bank(), bank()
                    wv4 = wukv.t[:].rearrange("p k (h c) -> p k h c", c=256)
                    with s.grp("pe", [latn.b, wukv.b], [pv0.b, pv1.b]) as g:
                        for hb in range(2):
                            pv = pv0 if hb == 0 else pv1
                            for c in range(2):
                                g.last = nc.tensor.matmul(pv.t[:], lhsT=latn.t[:, c, :], rhs=wv4[:, c, hb * 4:hb * 4 + 4, 128:256],
                                                          start=(c == 0), stop=(c == 1))
                    s.op("dve", lambda: V.tensor_copy(out=vs.t[:, 0:4, :], in_=pv0.t[:]), [pv0.b], [vs.b])
                    s.op("dve", lambda: V.tensor_copy(out=vs.t[:, 4:8, :], in_=pv1.t[:]), [pv1.b, vs.b], [vs.b])
                    s.dma("sp", VS[i], vs.t[:], reads=[vs.b], writes=[bVS[i]])
                    if i + 1 < NT:
                        norm_front(i + 1)
                        norm_back(i + 1, 0, hTs[(i + 1) % 2])
                    _sub(5)
                    if own:
                        pb = bank()
                        with s.grp("pe", [hT.b, hT.b2, winb["cq"]], [pb.b]) as g:
                            for c in range(4):
                                g.last = proj_fm(pb, c, O_CQ + c * 128, 128)
                        latent_norm(pb, 4, 512)
                        pq0, pq1 = bank(), bank()
                        with s.grp("pe", [latn.b, wuq.b], [pq0.b, pq1.b]) as g:
                            for h in range(8):
                                pq = pq0 if h < 4 else pq1
                                for c in range(4):
                                    g.last = nc.tensor.matmul(pq.t[:, h % 4, :], lhsT=wuq.t[:, c, h * 192:h * 192 + 128],
                                                              rhs=latn.t[:, c, :], start=(c == 0), stop=(c == 3))
                        s.op("act", lambda: A.copy(out=qNs.t[:, 0:4, :], in_=pq0.t[:]), [pq0.b], [qNs.b])
                        s.op("act", lambda: A.copy(out=qNs.t[:, 4:8, :], in_=pq1.t[:]), [pq1.b, qNs.b], [qNs.b])
                        for hb in range(2):
                            pa, pbb = bank(), bank()
                            with s.grp("pe", [latn.b, wuq.b, wqsw.b], [pa.b, pbb.b]) as g:
                                for hh in range(4):
                                    h = hb * 4 + hh
                                    for c in range(4):
                                        nc.tensor.matmul(pa.t[0:64, hh, :], lhsT=wuq.t[:, c, h * 192 + 128:h * 192 + 192],
                                                         rhs=latn.t[:, c, :], start=(c == 0), stop=(c == 3))
                                    for c in range(4):
                                        g.last = nc.tensor.matmul(pbb.t[0:64, hh, :], lhsT=wqsw.t[:, c, h, :],
                                                                  rhs=latn.t[:, c, :], start=(c == 0), stop=(c == 3))
                            hs = slice(hb * 4, hb * 4 + 4)
                            s.op("dve", lambda: V.tensor_tensor(out=rp1.t[:], in0=pa.t[0:64, :, :],
                                                                in1=r_i.t[:, 0:1, :].to_broadcast([64, 4, 128]), op=ALU.mult),
                                 [pa.b, r_i.b], [rp1.b])
                            s.op("dve", lambda: V.tensor_tensor(out=rp2.t[:], in0=pbb.t[0:64, :, :],
                                                                in1=r_i.t[:, 1:2, :].to_broadcast([64, 4, 128]), op=ALU.mult),
                                 [pbb.b, r_i.b], [rp2.b])
                            s.op("pool", lambda: P.tensor_tensor(out=qRs.t[:, hs, :], in0=rp1.t[:], in1=rp2.t[:], op=ALU.add),
                                 [rp1.b, rp2.b], [qRs.b])
                        s.dma("sp", QN[j], qNs.t[:], reads=[qNs.b], writes=[bQ[j]])
                        s.dma("sp", QR[j], qRs.t[:], reads=[qRs.b], writes=[bQ[j]])
                    pg = bank()
                    s.op("pe", lambda: proj_fm(pg, 0, O_GLR, 16), [hT.b, hT.b2, winb["glr"]], [pg.b])
                    s.op("act", lambda: A.copy(out=glrT.t[0:16, :], in_=pg.t[0:16, 0, :]), [pg.b], [glrT.b])
                    ppre = bank()
                    s.op("pe", lambda: nc.tensor.matmul(fl(ppre), lhsT=glrT.t[:], rhs=w2a.t[:], start=True, stop=True),
                         [glrT.b, w2a.b], [ppre.b])
                    s.op("act", lambda: A.activation(out=la.t[:], in_=ppre.t[:].rearrange("p a b -> p (a b)"), func=AF.Exp, scale=-1.0),
                         [ppre.b], [la.b])
                    s.op("act", lambda: A.activation(out=la.t[:], in_=la.t[:], func=AF.Ln, bias=1.0), [la.b], [la.b])
                    fcol = C_FLAG if i == 0 else C_ONE
                    s.op("dve", lambda: V.tensor_scalar(out=la.t[:], in0=la.t[:], scalar1=-1.0 / 16.0,
                                                        scalar2=vecs.t[:, fcol:fcol + 1], op0=ALU.mult, op1=ALU.mult),
                         [la.b, vecs.b], [la.b])
                    pgk = bank()
                    with s.grp("pe", [hT.b, hT.b2, winb["gk"]], [pgk.b]) as g:
                        for k in range(16):
                            g.last = nc.tensor.matmul(fl(pgk), lhsT=hT.t[:, k, :], rhs=win.t[:, k, O_GK:O_GK + 512],
                                                      start=(k == 0), stop=(k == 15))
                    s.op("act", lambda: A.copy(out=gktm.t[:], in_=pgk.t[:].rearrange("p a b -> p (a b)")), [pgk.b], [gktm.b])
                    pgv0, pgv1 = bank(), bank()
                    with s.grp("pe", [hT.b, hT.b2, winb["gv"]], [pgv0.b, pgv1.b]) as g:
                        for hb in range(2):
                            pv = pgv0 if hb == 0 else pgv1
                            for k in range(16):
                                g.last = nc.tensor.matmul(fl(pv), lhsT=hT.t[:, k, :],
                                                          rhs=win.t[:, k, O_GV + hb * 512:O_GV + (hb + 1) * 512],
                                                          start=(k == 0), stop=(k == 15))
                    s.op("dve", lambda: V.tensor_copy(out=gvb.t[:, 0:512], in_=pgv0.t[:].rearrange("p a b -> p (a b)")), [pgv0.b], [gvb.b])
                    s.op("dve", lambda: V.tensor_copy(out=gvb.t[:, 512:1024], in_=pgv1.t[:].rearrange("p a b -> p (a b)")), [pgv1.b, gvb.b], [gvb.b])
                    pr3 = bank()
                    s.op("pe", lambda: nc.tensor.matmul(fl(pr3), lhsT=cm.t[:, 4, :], rhs=la.t[:], start=True, stop=True),
                         [cm.b, la.b], [pr3.b])
                    s.op("act", lambda: A.activation(out=e3.t[:], in_=pr3.t[:].rearrange("p a b -> p (a b)"), func=AF.Exp), [pr3.b], [e3.b])
                    pbl = bank()
                    with s.grp("pe", [la.b, onesf.b], [pbl.b]) as g:
                        for h in range(4):
                            g.last = nc.tensor.matmul(pbl.t[:, 0, h:h + 1], lhsT=la.t[:, h * 128:(h + 1) * 128], rhs=onesf.t[:, 0:1],
                                                      start=True, stop=True)
                    s.op("act", lambda: A.activation(out=dec.t[:], in_=pbl.t[:, 0, 0:4], func=AF.Exp), [pbl.b], [dec.b])
                    s.op("dve", lambda: V.scalar_tensor_tensor(out=ke2.t[:], in0=gktm.t[:], scalar=vecs.t[:, fcol:fcol + 1],
                                                               in1=e3.t[:], op0=ALU.mult, op1=ALU.mult),
                         [gktm.b, e3.b, vecs.b], [ke2.b])
                    if own:
                        pgq = bank()
                        with s.grp("pe", [hT.b, hT.b2, winb["gq"]], [pgq.b]) as g:
                            for h in range(4):
                                g.last = proj_fm(pgq, h, O_GQ + h * 128, 128)
                        pgkT = bank()
                        with s.grp("pe", [hT.b, hT.b2, winb["gk"]], [pgkT.b]) as g:
                            for h in range(4):
                                g.last = proj_fm(pgkT, h, O_GK + h * 128, 128)
                        pc1, pbc = bank(), bank()
                        with s.grp("pe", [la.b, cm.b], [pc1.b, pbc.b]) as g:
                            for h in range(4):
                                nc.tensor.matmul(pc1.t[:, h, :], lhsT=la.t[:, h * 128:(h + 1) * 128], rhs=cm.t[:, 2, :], start=True, stop=True)
                                g.last = nc.tensor.matmul(pbc.t[:, h, :], lhsT=la.t[:, h * 128:(h + 1) * 128], rhs=cm.t[:, 3, :], start=True, stop=True)
                        s.op("act", lambda: A.activation(out=e1.t[:], in_=pc1.t[:], func=AF.Exp), [pc1.b], [e1.b])
                        s.op("act", lambda: A.activation(out=e2.t[:], in_=pc1.t[:], func=AF.Exp, scale=-1.0), [pc1.b], [e2.b])
                        s.op("act", lambda: A.activation(out=e4.t[:], in_=pbc.t[:], func=AF.Exp), [pbc.b], [e4.b])
                        sc = 128.0 ** -0.5
                        s.op("dve", lambda: V.scalar_tensor_tensor(out=qeT.t[:], in0=pgq.t[:], scalar=sc, in1=e1.t[:],
                                                                   op0=ALU.mult, op1=ALU.mult), [pgq.b, e1.b], [qeT.b])
                        s.op("dve", lambda: V.scalar_tensor_tensor(out=qbT.t[:], in0=pgq.t[:], scalar=sc, in1=e4.t[:],
                                                                   op0=ALU.mult, op1=ALU.mult), [pgq.b, e4.b], [qbT.b])
                        s.op("dve", lambda: V.tensor_tensor(out=keT.t[:], in0=pgkT.t[:], in1=e2.t[:], op=ALU.mult),
                             [pgkT.b, e2.b], [keT.b])
                        pat = bank()
                        with s.grp("pe", [keT.b, qeT.b], [pat.b]) as g:
                            for h in range(4):
                                g.last = nc.tensor.matmul(pat.t[:, h, :], lhsT=keT.t[:, h, :], rhs=qeT.t[:, h, :], start=True, stop=True)
                        s.op("dve", lambda: V.tensor_tensor(out=ATm.t[:], in0=pat.t[:],
                                                            in1=cm.t[:, 1:2, :].to_broadcast([128, 4, 128]), op=ALU.mult),
                             [pat.b, cm.b], [ATm.b])
                        po0, po1 = bank(), bank()
                        with s.grp("pe", [Sbf.b, qbT.b, gvb.b, ATm.b], [po0.b, po1.b]) as g:
                            for h in range(4):
                                for c in range(2):
                                    po = po0 if h < 2 else po1
                                    slot = (h % 2) * 2 + c
                                    nc.tensor.matmul(po.t[:, slot, :], lhsT=Sbf.t[:, h, c * 128:(c + 1) * 128], rhs=qbT.t[:, h, :],
                                                     start=True, stop=False)
                                    g.last = nc.tensor.matmul(po.t[:, slot, :], lhsT=gvb.t[:, h * 256 + c * 128:h * 256 + (c + 1) * 128],
                                                              rhs=ATm.t[:, h, :], start=False, stop=True)
                        for hb, po in enumerate((po0, po1)):
                            s.op("act", lambda: A.activation(out=ogsq.t[:, hb * 4:hb * 4 + 4, :], in_=po.t[:], func=AF.Square), [po.b], [ogsq.b])
                            s.op("dve", lambda: V.tensor_copy(out=ogr.t[:, hb * 4:hb * 4 + 4, :], in_=po.t[:]), [po.b], [ogr.b])
                        pss = bank()
                        with s.grp("pe", [ogsq.b, onesb.b], [pss.b]) as g:
                            for h in range(4):
                                for c in range(2):
                                    g.last = nc.tensor.matmul(pss.t[:, h, :], lhsT=onesb.t[:], rhs=ogsq.t[:, h * 2 + c, :],
                                                              start=(c == 0), stop=(c == 1))
                        rstd_from(None, pss.t[:], 256, [pss.b], rsb, rsb.t[:])
                        og4 = ogr.t[:].rearrange("p (h c) t -> p h c t", c=2)
                        s.op("dve", lambda: V.tensor_tensor(out=og4, in0=og4, in1=rsb.t[:, :, None, :].to_broadcast([128, 4, 2, 128]),
                                                            op=ALU.mult), [ogr.b, rsb.b], [ogr.b])
                        s.op("pool", lambda: P.tensor_tensor(out=mixg.t[:], in0=ogr.t[:],
                                                             in1=vecs.t[:, C_GOG:C_GOG + 8, None].to_broadcast([128, 8, 128]),
                                                             op=ALU.mult), [ogr.b, vecs.b], [mixg.b])
                        s.dma("sp", MIX[j, :, 8:16, :], mixg.t[:], reads=[mixg.b], writes=[bMIXg[j]])
                    pu0, pu1 = bank(), bank()
                    with s.grp("pe", [ke2.b, gvb.b], [pu0.b, pu1.b]) as g:
                        for h in range(4):
                            pu = pu0 if h < 2 else pu1
                            g.last = nc.tensor.matmul(pu.t[:, (h % 2) * 2:(h % 2) * 2 + 2, :].rearrange("p a b -> p (a b)"), lhsT=ke2.t[:, h * 128:(h + 1) * 128],
                                                      rhs=gvb.t[:, h * 256:(h + 1) * 256], start=True, stop=True)
                    for h in range(4):
                        pu = pu0 if h < 2 else pu1
                        s.op("dve", lambda: V.scalar_tensor_tensor(out=Sst.t[:, h, :], in0=Sst.t[:, h, :], scalar=dec.t[:, h:h + 1],
                                                                   in1=pu.t[:, (h % 2) * 2:(h % 2) * 2 + 2, :].rearrange("p a b -> p (a b)"),
                                                                   op0=ALU.mult, op1=ALU.add), [Sst.b, dec.b, pu.b], [Sst.b])
                    s.op("act", lambda: A.copy(out=Sbf.t[:], in_=Sst.t[:]), [Sst.b], [Sbf.b])
            except _StopDbg:
                pass

        s.barrier()
        with ExitStack() as p2:
            KNs = sbt(p2, "KNs", [128, NT, 8, 128], BF16)
            VSs = sbt(p2, "VSs", [128, NT, 8, 128], BF16)
            KRs = sbt(p2, "KRs", [128, NT, 128], BF16)
            bk = [Buf() for _ in range(NT)]
            s.op("pool", lambda: P.memset(KRs.t[64:128, :, :], 0.0), [], [KRs.b])
            bkN = [Buf() for _ in range(NT)]
            bkV = [Buf() for _ in range(NT)]
            for i0, i1 in ((0, 2), (2, 8), (8, 16), (16, 24), (24, 32)):
                grp_ = list(range(i0, i1))
                s.dma("sp", KNs.t[:, i0:i1], KN[i0:i1].rearrange("i p h t -> p i h t"), reads=[bKN[i] for i in grp_], writes=[bkN[i] for i in grp_])
                s.dma("sp", KRs.t[0:64, i0:i1, :], KR[i0:i1].rearrange("i p t -> p i t"), reads=[bKR[i] for i in grp_] + [KRs.b], writes=[bk[i] for i in grp_])
                s.dma("sp", VSs.t[:, i0:i1], VS[i0:i1].rearrange("i p h t -> p i h t"), reads=[bVS[i] for i in grp_], writes=[bkV[i] for i in grp_])
            PS = [[pst(p2, "PS%d%d" % (a_, b_), [128, 4, 128]) for b_ in range(2)] for a_ in range(2)]
            PO = [pst(p2, "PO%d" % b_, [128, 4, 128]) for b_ in range(2)]
            PL = [pst(p2, "PL%d" % b_, [128, 4, 128]) for b_ in range(2)]
            qn = [sbt(p2, "qn%d" % i, [128, 8, 128], BF16) for i in range(2)]
            qr = [sbt(p2, "qr%d" % i, [128, 8, 128], BF16) for i in range(2)]
            pT = [sbt(p2, "pT%d" % i, [128, 8, 128], BF16) for i in range(2)]
            Lacc = sbt(p2, "Lacc", [128, 8, 128], F32)
            Lr = sbt(p2, "Lr", [128, 8, 128], F32)
            of = sbt(p2, "of", [128, 8, 128], F32)
            osq = sbt(p2, "osq", [128, 8, 128], BF16)
            rs8 = sbt(p2, "rs8", [128, 8, 128], F32)
            mixm = sbt(p2, "mixm", [128, 8, 128], BF16)
            for i in range(2):
                s.op("pool", lambda: P.memset(qr[i].t[64:128, :, :], 0.0), [], [qr[i].b])
            scale = 192.0 ** -0.5
            LaccD = sbt(p2, "LaccD", [128, 8, 128], F32)
            pairs = [(j, kt) for j in range(NOWN if stop >= 3 else 0) for kt in range(2 * j + 2)]

            def emit_S(n):
                j, kt = pairs[n]
                qn_j, qr_j = qn[j % 2], qr[j % 2]
                if kt == 0:
                    s.dma("sp", qn_j.t[:], QN[j], reads=[bQ[j]], writes=[qn_j.b])
                    s.dma("sp", qr_j.t[0:64], QR[j], reads=[bQ[j], qr_j.b], writes=[qr_j.b2])
                ps = PS[n % 2]
                with s.grp("pe", [bk[kt], bkN[kt], KRs.b, qn_j.b, qr_j.b, qr_j.b2], [ps[0].b, ps[1].b]) as g:
                    for h in range(8):
                        nc.tensor.matmul(ps[h // 4].t[:, h % 4, :], lhsT=KNs.t[:, kt, h, :], rhs=qn_j.t[:, h, :], start=True, stop=False)
                        g.last = nc.tensor.matmul(ps[h // 4].t[:, h % 4, :], lhsT=KRs.t[:, kt, :], rhs=qr_j.t[:, h, :], start=False, stop=True)

            if pairs:
                emit_S(0)
            for n, (j, kt) in enumerate(pairs):
                io = 2 * j + 1
                ps, pt_ = PS[n % 2], pT[n % 2]
                if n + 1 < len(pairs):
                    emit_S(n + 1)
                for hb in range(2):
                    if kt == 0:
                        s.op("act", lambda: A.activation(out=pt_.t[:, hb * 4:hb * 4 + 4, :], in_=ps[hb].t[:], func=AF.Exp, scale=scale,
                                                         bias=vecs.t[:, C_NEG:C_NEG + 1]), [ps[hb].b, vecs.b], [pt_.b])
                    else:
                        s.op("act", lambda: A.activation(out=pt_.t[:, hb * 4:hb * 4 + 4, :], in_=ps[hb].t[:], func=AF.Exp, scale=scale),
                             [ps[hb].b], [pt_.b])
                if kt == io:
                    s.op("pool", lambda: P.tensor_tensor(out=pt_.t[:], in0=pt_.t[:],
                                                         in1=trib.t[:, None, :].to_broadcast([128, 8, 128]), op=ALU.mult),
                         [pt_.b, trib.b], [pt_.b])
                if kt % 2 == 0:
                    if kt == 0:
                        s.op("pool", lambda: P.tensor_copy(out=Lacc.t[:], in_=pt_.t[:]), [pt_.b], [Lacc.b])
                    else:
                        s.op("pool", lambda: P.tensor_tensor(out=Lacc.t[:], in0=Lacc.t[:], in1=pt_.t[:], op=ALU.add),
                             [Lacc.b, pt_.b], [Lacc.b])
                else:
                    if kt == 1:
                        s.op("dve", lambda: V.tensor_copy(out=LaccD.t[:], in_=pt_.t[:]), [pt_.b], [LaccD.b])
                    else:
                        s.op("dve", lambda: V.tensor_tensor(out=LaccD.t[:], in0=LaccD.t[:], in1=pt_.t[:], op=ALU.add),
                             [LaccD.b, pt_.b], [LaccD.b])
                with s.grp("pe", [bkV[kt], pt_.b], [PO[0].b, PO[1].b]) as g:
                    for h in range(8):
                        st_ = (kt == 0 and h % 4 == 0)
                        g.last = nc.tensor.matmul(PO[h // 4].t[:, h % 4, :], lhsT=VSs.t[:, kt, h, :], rhs=pt_.t[:, h, :],
                                                  start=st_, stop=(kt == io and h % 4 == 3), skip_group_check=True)
                if kt != io:
                    continue
                with s.grp("pe", [Lacc.b, LaccD.b, onesf.b], [PL[0].b, PL[1].b]) as g:
                    for hb in range(2):
                        nc.tensor.matmul(fl(PL[hb]), lhsT=onesf.t[:], rhs=Lacc.t[:, hb * 4:hb * 4 + 4, :].rearrange("p a b -> p (a b)"),
                                         start=True, stop=False)
                        g.last = nc.tensor.matmul(fl(PL[hb]), lhsT=onesf.t[:], rhs=LaccD.t[:, hb * 4:hb * 4 + 4, :].rearrange("p a b -> p (a b)"),
                                                  start=False, stop=True)
                for hb in range(2):
                    hs = slice(hb * 4, hb * 4 + 4)
                    s.op("dve", lambda: V.reciprocal(out=Lr.t[:, hs, :], in_=PL[hb].t[:]), [PL[hb].b], [Lr.b])
                    s.op("dve", lambda: V.tensor_tensor(out=of.t[:, hs, :], in0=PO[hb].t[:], in1=Lr.t[:, hs, :], op=ALU.mult),
                         [PO[hb].b, Lr.b], [of.b])
                s.op("act", lambda: A.activation(out=osq.t[:], in_=of.t[:], func=AF.Square), [of.b], [osq.b])
                with s.grp("pe", [osq.b, onesb.b], [PL[0].b, PL[1].b]) as g:
                    for h in range(8):
                        g.last = nc.tensor.matmul(PL[h // 4].t[:, h % 4, :], lhsT=onesb.t[:], rhs=osq.t[:, h, :], start=True, stop=True)
                for hb in range(2):
                    rstd_from(None, PL[hb].t[:], 128, [PL[hb].b], rs8, rs8.t[:, hb * 4:hb * 4 + 4, :])
                s.op("dve", lambda: V.tensor_tensor(out=of.t[:], in0=of.t[:], in1=rs8.t[:], op=ALU.mult), [of.b, rs8.b], [of.b])
                s.op("pool", lambda: P.tensor_tensor(out=mixm.t[:], in0=of.t[:],
                                                     in1=vecs.t[:, C_MOG:C_MOG + 8, None].to_broadcast([128, 8, 128]), op=ALU.mult),
                     [of.b, vecs.b], [mixm.b])
                s.dma("sp", MIX[j, :, 0:8, :], mixm.t[:], reads=[mixm.b], writes=[bMIXm[j]])


        s.barrier()
        with ExitStack() as p3:
            skf = sbt(p3, "skf", [128, 16, 128], F32)
            skb = sbt(p3, "skb", [128, 16, 128], BF16)
            PT = pst(p3, "PT3s", [128, 16, 128], BF16)
            s.dma("sp", skf.t[:], subk.rearrange("c k d -> k c d"), writes=[skf.b])
            s.op("dve", lambda: V.tensor_copy(out=skb.t[:], in_=skf.t[:]), [skf.b], [skb.b])
            with s.grp("pe", [skb.b, identb.b], [PT.b]) as g:
                for c in range(16):
                    g.last = nc.tensor.transpose(PT.t[:, c, :], skb.t[:, c, :], identb.t[:])
            s.op("dve", lambda: V.tensor_copy(out=skT.t[:], in_=PT.t[:]), [PT.b], [skT.b])

        s.barrier()
        with ExitStack() as p3:
            wo = sbt(p3, "wo", [128, 16, D], BF16)
            wgr = sbt(p3, "wgr", [128, 16, 1024], BF16)
            hb1 = [sbt(p3, "hb1%d" % i, [128, 16, 128], BF16) for i in range(2)]
            sg = sbt(p3, "sg", [128, 8, 128], BF16)
            g1b = sbt(p3, "g1b", [128, D], F32)
            w_out_v = w_out.rearrange("(k p) n -> p k n", p=128)
            w_in_v = w_in.rearrange("(k p) n -> p k n", p=128)
            wgrb = [Buf() for _ in range(2)]
            wob = [Buf() for _ in range(4)]
            for hb in range(2):
                s.dma("pool", wgr.t[:, :, hb * 512:(hb + 1) * 512], w_in_v[:, :, O_GR + hb * 512:O_GR + (hb + 1) * 512], writes=[wgrb[hb]])
            for blk in range(4):
                s.dma("pool", wo.t[:, :, blk * 512:(blk + 1) * 512], w_out_v[:, :, blk * 512:(blk + 1) * 512], writes=[wob[blk]])
            s.dma("sp", g1b.t[:], G12[0], reads=[bG12], writes=[g1b.b])
            PT = pst(p3, "PT3", [128, 16, 128], BF16)
            PB = [pst(p3, "PB3%d" % i, [128, 4, 128]) for i in range(6)]
            pbi = [0]

            def bank3():
                b = PB[pbi[0] % 6]
                pbi[0] += 1
                return b
            mx = [sbt(p3, "mx%d" % i, [128, 16, 128], BF16) for i in range(2)]
            x3 = [sbt(p3, "x3%d" % i, [128, D], F32) for i in range(2)]
            x2 = [sbt(p3, "x2t%d" % i, [128, D], F32) for i in range(2)]
            junk = sbt(p3, "junk3", [128, D], BF16)
            xn = sbt(p3, "xn3", [128, D], BF16)
            h2 = [sbt(p3, "h2%d" % i, [128, 16, 128], BF16) for i in range(2)]
            st1 = sbt(p3, "st13", [128, 4], F32)

            def load3(j):
                s.dma("sp", hb1[j % 2].t[:], HT1[j], reads=[bHT1[j]], writes=[hb1[j % 2].b])
                s.dma("sp", mx[j % 2].t[:], MIX[j], reads=[bMIXm[j], bMIXg[j]], writes=[mx[j % 2].b])
                s.dma("sp", x3[j % 2].t[:], xs[(2 * j + 1) * 128:(2 * j + 2) * 128, :], writes=[x3[j % 2].b])
            load3(0)
            for j in range(NOWN if stop >= 4 else 0):
                mxj, xj, x2j, h2j = mx[j % 2], x3[j % 2], x2[j % 2], h2[j % 2]
                hj = hb1[j % 2]
                if j + 1 < NOWN:
                    load3(j + 1)
                for hb in range(2):
                    pgr = bank3()
                    with s.grp("pe", [hj.b, wgrb[hb]], [pgr.b]) as g:
                        for c in range(4):
                            col0 = (hb * 4 + c) * 128
                            for k in range(16):
                                g.last = nc.tensor.matmul(pgr.t[:, c, :], lhsT=wgr.t[:, k, col0:col0 + 128], rhs=hj.t[:, k, :],
                                                          start=(k == 0), stop=(k == 15))
                    s.op("act", lambda: A.activation(out=sg.t[:, hb * 4:hb * 4 + 4, :], in_=pgr.t[:], func=AF.Silu), [pgr.b], [sg.b])
                s.op("dve", lambda: V.tensor_tensor(out=mxj.t[:, 8:16, :], in0=mxj.t[:, 8:16, :], in1=sg.t[:], op=ALU.mult),
                     [mxj.b, sg.b], [mxj.b])
                for blk in range(4):
                    pb = bank3()
                    with s.grp("pe", [mxj.b, wob[blk]], [pb.b]) as g:
                        for k in range(16):
                            g.last = nc.tensor.matmul(fl(pb), lhsT=mxj.t[:, k, :], rhs=wo.t[:, k, blk * 512:(blk + 1) * 512],
                                                      start=(k == 0), stop=(k == 15))
                    bs = slice(blk * 512, (blk + 1) * 512)
                    s.op("dve", lambda: V.tensor_tensor(out=x2j.t[:, bs], in0=fl(pb), in1=g1b.t[:, bs], op=ALU.mult),
                         [pb.b, g1b.b], [x2j.b])
                s.op("pool", lambda: P.tensor_tensor(out=x2j.t[:], in0=x2j.t[:], in1=xj.t[:], op=ALU.add),
                     [x2j.b, xj.b], [x2j.b])
                s.dma("sp", X2[j], x2j.t[:], reads=[x2j.b], writes=[bX2[j]])
                s.op("act", lambda: A.activation(out=junk.t[:], in_=x2j.t[:], func=AF.Square, accum_out=st1.t[:, 0:1]),
                     [x2j.b], [junk.b, st1.b])
                rstd_from(None, st1.t[:, 0:1], D, [st1.b], st1, st1.t[:, 1:2])
                s.op("act", lambda: A.activation(out=xn.t[:], in_=x2j.t[:], func=AF.Copy, scale=st1.t[:, 1:2]),
                     [x2j.b, st1.b], [xn.b])
                with s.grp("pe", [xn.b, identb.b], [PT.b]) as g:
                    for k in range(16):
                        g.last = nc.tensor.transpose(PT.t[:, k, :], xn.t[:, k * 128:(k + 1) * 128], identb.t[:])
                for k in range(16):
                    if k % 2 == 0:
                        s.op("dve", lambda: V.tensor_scalar(out=h2j.t[:, k, :], in0=PT.t[:, k, :], scalar1=AB.t[:, 32 + k:33 + k],
                                                            scalar2=AB.t[:, 48 + k:49 + k], op0=ALU.mult, op1=ALU.add),
                             [PT.b, AB.b], [h2j.b])
                    else:
                        s.op("act", lambda: A.activation(out=h2j.t[:, k, :], in_=PT.t[:, k, :], func=AF.Identity,
                                                         scale=AB.t[:, 32 + k:33 + k], bias=AB.t[:, 48 + k:49 + k]),
                             [PT.b, AB.b], [h2j.b2])
                s.dma("sp", H2T[j], h2j.t[:], reads=[h2j.b, h2j.b2], writes=[bH2T[j]])

        s.barrier()
        with ExitStack() as p3:
            U32 = mybir.dt.uint32
            wq = sbt(p3, "wq", [128, 16, D], BF16)
            w_pq_v = w_pq.rearrange("(k p) n -> p k n", p=128)
            wqb = [Buf() for _ in range(4)]
            for cb in range(4):
                s.dma("pool", wq.t[:, :, cb * 512:(cb + 1) * 512], w_pq_v[:, :, cb * 512:(cb + 1) * 512], writes=[wqb[cb]])
            PB = [pst(p3, "PB4%d" % i, [128, 4, 128]) for i in range(6)]
            PT = pst(p3, "PT4", [128, 8, 128], BF16)
            pbi = [0]

            def bank3():
                b = PB[pbi[0] % 6]
                pbi[0] += 1
                return b
            h2 = [sbt(p3, "h2b%d" % i, [128, 16, 128], BF16) for i in range(2)]
            qpT = sbt(p3, "qpT", [128, 16, 128], BF16)
            sall = [sbt(p3, "sall%d" % i, [128, 16, 128], F32) for i in range(2)]
            wk = sbt(p3, "wk", [128, 256], F32)
            a16 = sbt(p3, "a16", [128, 16, 16], F32)
            i16u = sbt(p3, "i16u", [128, 16, 16], U32)
            i16 = sbt(p3, "i16", [128, 16, 16], F32)
            cand = sbt(p3, "cand", [128, 8, 256], F32)
            best = sbt(p3, "best", [128, 8, 16], F32)
            bixu = sbt(p3, "bixu", [128, 8, 16], U32)
            bix = sbt(p3, "bix", [128, 8, 16], F32)
            mm = sbt(p3, "mm", [128, 16], F32)
            eb = sbt(p3, "eb", [128, 8, 16], F32)
            u17 = sbt(p3, "u17", [128, 8, 16, 17], F32)
            sel = sbt(p3, "sel", [128, 8, 16, 16], F32)
            jv = sbt(p3, "jv", [128, 8, 16], F32)
            kv_ = sbt(p3, "kv_", [128, 8, 16], F32)
            lists = sbt(p3, "lists", [128, 3, 128], F32)
            listsb = sbt(p3, "listsb", [128, 3, 128], BF16)
            pmj = [sbt(p3, "pmaj%d" % i, [128, 3, 128], BF16) for i in range(2)]
            io17 = sbt(p3, "io17", [128, 17], F32)
            io128 = sbt(p3, "io128", [128, 128], F32)
            io128b = sbt(p3, "io128b", [128, 128], BF16)
            TH = 64
            Rm = sbt(p3, "Rm", [128, TH, 128], BF16)
            OH = sbt(p3, "OH", [128, TH, 128], BF16)
            wTt = sbt(p3, "wTt", [128, 128, 128], BF16)
            io128i = sbt(p3, "io128i", [128, 128], I32)
            s.op("pool", lambda: P.iota(io128i.t[:], pattern=[[1, 128]], base=0, channel_multiplier=0), [], [io128i.b])
            s.op("dve", lambda: V.tensor_copy(out=io128.t[:], in_=io128i.t[:]), [io128i.b], [io128.b])
            s.op("dve", lambda: V.tensor_copy(out=io128b.t[:], in_=io128.t[:]), [io128.b], [io128b.b])
            s.op("dve", lambda: V.tensor_scalar(out=io17.t[:], in0=io128.t[:, 0:17], scalar1=16.0, scalar2=None, op0=ALU.mult),
                 [io128.b], [io17.b])
            s.dma("sp", h2[0].t[:], H2T[0], reads=[bH2T[0]], writes=[h2[0].b])
            def stageQ(j):
                h2j, sal_ = h2[j % 2], sall[j % 2]
                if j + 1 < NOWN:
                    s.dma("sp", h2[(j + 1) % 2].t[:], H2T[j + 1], reads=[bH2T[j + 1]], writes=[h2[(j + 1) % 2].b])
                for cb in range(4):
                    pb = bank3()
                    with s.grp("pe", [h2j.b, wqb[cb]], [pb.b]) as g:
                        for cc in range(4):
                            c = cb * 4 + cc
                            for k in range(16):
                                g.last = nc.tensor.matmul(pb.t[:, cc, :], lhsT=wq.t[:, k, c * 128:(c + 1) * 128], rhs=h2j.t[:, k, :],
                                                          start=(k == 0), stop=(k == 15))
                    s.op("act", lambda: A.copy(out=qpT.t[:, cb * 4:cb * 4 + 4, :], in_=pb.t[:]), [pb.b], [qpT.b])
                for cb in range(4):
                    pb = bank3()
                    with s.grp("pe", [qpT.b, skT.b], [pb.b]) as g:
                        for cc in range(4):
                            c = cb * 4 + cc
                            g.last = nc.tensor.matmul(pb.t[:, cc, :], lhsT=qpT.t[:, c, :], rhs=skT.t[:, c, :], start=True, stop=True)
                    s.op("act", lambda: A.copy(out=sal_.t[:, cb * 4:cb * 4 + 4, :], in_=pb.t[:]), [pb.b], [sal_.b])
            def stageT(j):
                sal_, pmaj = sall[j % 2], pmj[j % 2]
                for c in range(16):
                    s.op("dve", lambda: V.max(out=a16.t[:, c, 0:8], in_=sal_.t[:, c, :]), [sal_.b], [a16.b])
                    s.op("dve", lambda: V.max_index(out=i16u.t[:, c, 0:8], in_max=a16.t[:, c, 0:8], in_values=sal_.t[:, c, :]),
                         [sal_.b, a16.b], [i16u.b])
                    s.op("dve", lambda: V.match_replace(out=wk.t[:, 0:128], in_to_replace=a16.t[:, c, 0:8], in_values=sal_.t[:, c, :],
                                                        imm_value=-1e30), [sal_.b, a16.b], [wk.b])
                    s.op("dve", lambda: V.max(out=a16.t[:, c, 8:16], in_=wk.t[:, 0:128]), [wk.b], [a16.b])
                    s.op("dve", lambda: V.max_index(out=i16u.t[:, c, 8:16], in_max=a16.t[:, c, 8:16], in_values=wk.t[:, 0:128]),
                         [wk.b, a16.b], [i16u.b])
                s.op("dve", lambda: V.tensor_copy(out=i16.t[:], in_=i16u.t[:]), [i16u.b], [i16.b])
                a4 = a16.t[:].rearrange("p (h two) k -> p h two k", two=2)
                i4 = i16.t[:].rearrange("p (h two) k -> p h two k", two=2)
                c4 = cand.t[:].rearrange("p h (a b) -> p h a b", b=16)
                s.op("dve", lambda: V.tensor_tensor(out=c4, in0=a4[:, :, 0, :, None].to_broadcast([128, 8, 16, 16]),
                                                    in1=a4[:, :, 1, None, :].to_broadcast([128, 8, 16, 16]), op=ALU.add),
                     [a16.b], [cand.b])
                for h in range(8):
                    s.op("dve", lambda: V.max(out=best.t[:, h, 0:8], in_=cand.t[:, h, :]), [cand.b], [best.b])
                    s.op("dve", lambda: V.max_index(out=bixu.t[:, h, 0:8], in_max=best.t[:, h, 0:8], in_values=cand.t[:, h, :]),
                         [cand.b, best.b], [bixu.b])
                    s.op("dve", lambda: V.match_replace(out=wk.t[:], in_to_replace=best.t[:, h, 0:8], in_values=cand.t[:, h, :],
                                                        imm_value=-1e30), [cand.b, best.b], [wk.b])
                    s.op("dve", lambda: V.max(out=best.t[:, h, 8:16], in_=wk.t[:]), [wk.b], [best.b])
                    s.op("dve", lambda: V.max_index(out=bixu.t[:, h, 8:16], in_max=best.t[:, h, 8:16], in_values=wk.t[:]),
                         [wk.b, best.b], [bixu.b])
                s.op("dve", lambda: V.tensor_copy(out=bix.t[:], in_=bixu.t[:]), [bixu.b], [bix.b])
                s.op("dve", lambda: V.tensor_copy(out=mm.t[:, 0:8], in_=best.t[:, :, 0]), [best.b], [mm.b])
                s.op("dve", lambda: V.tensor_tensor(out=eb.t[:], in0=best.t[:], in1=mm.t[:, 0:8, None].to_broadcast([128, 8, 16]),
                                                    op=ALU.subtract), [best.b, mm.b], [eb.b])
                s.op("act", lambda: A.activation(out=eb.t[:], in_=eb.t[:], func=AF.Exp), [eb.b], [eb.b])
                s.op("dve", lambda: V.reduce_sum(out=mm.t[:, 8:16], in_=eb.t[:], axis=AX.X), [eb.b], [mm.b])
                s.op("dve", lambda: V.reciprocal(out=mm.t[:, 8:16], in_=mm.t[:, 8:16]), [mm.b], [mm.b])
                l3 = lists.t[:].rearrange("p a (h r) -> p a h r", r=16)
                s.op("dve", lambda: V.tensor_tensor(out=l3[:, 2], in0=eb.t[:], in1=mm.t[:, 8:16, None].to_broadcast([128, 8, 16]),
                                                    op=ALU.mult), [eb.b, mm.b], [lists.b])
                s.op("dve", lambda: V.tensor_tensor(out=u17.t[:], in0=bix.t[:, :, :, None].to_broadcast([128, 8, 16, 17]),
                                                    in1=io17.t[:, None, None, :].to_broadcast([128, 8, 16, 17]), op=ALU.is_ge),
                     [bix.b, io17.b], [u17.b])
                s.op("dve", lambda: V.tensor_tensor(out=sel.t[:], in0=u17.t[:, :, :, 0:16], in1=u17.t[:, :, :, 1:17], op=ALU.subtract),
                     [u17.b], [sel.b])
                s.op("dve", lambda: V.tensor_tensor(out=u17.t[:, :, :, 0:16], in0=sel.t[:], in1=i4[:, :, 0, None, :].to_broadcast([128, 8, 16, 16]),
                                                    op=ALU.mult), [sel.b, i16.b], [u17.b])
                s.op("dve", lambda: V.reduce_sum(out=l3[:, 0], in_=u17.t[:, :, :, 0:16], axis=AX.X), [u17.b], [lists.b])
                s.op("dve", lambda: V.tensor_tensor(out=u17.t[:, :, :, 0:16], in0=sel.t[:],
                                                    in1=io128.t[:, None, None, 0:16].to_broadcast([128, 8, 16, 16]), op=ALU.mult),
                     [sel.b, io128.b, u17.b], [u17.b])
                s.op("dve", lambda: V.reduce_sum(out=jv.t[:], in_=u17.t[:, :, :, 0:16], axis=AX.X), [u17.b], [jv.b])
                s.op("dve", lambda: V.scalar_tensor_tensor(out=kv_.t[:], in0=jv.t[:], scalar=-16.0, in1=bix.t[:], op0=ALU.mult, op1=ALU.add),
                     [jv.b, bix.b], [kv_.b])
                s.op("dve", lambda: V.tensor_tensor(out=sel.t[:], in0=kv_.t[:, :, :, None].to_broadcast([128, 8, 16, 16]),
                                                    in1=io128.t[:, None, None, 0:16].to_broadcast([128, 8, 16, 16]), op=ALU.is_equal),
                     [kv_.b, io128.b, sel.b], [sel.b])
                s.op("dve", lambda: V.tensor_tensor(out=u17.t[:, :, :, 0:16], in0=sel.t[:], in1=i4[:, :, 1, None, :].to_broadcast([128, 8, 16, 16]),
                                                    op=ALU.mult), [sel.b, i16.b, u17.b], [u17.b])
                s.op("dve", lambda: V.reduce_sum(out=l3[:, 1], in_=u17.t[:, :, :, 0:16], axis=AX.X), [u17.b], [lists.b])
                s.op("dve", lambda: V.tensor_copy(out=listsb.t[:], in_=lists.t[:]), [lists.b], [listsb.b])
                with s.grp("pe", [listsb.b, identb.b], [PT.b]) as g:
                    for a_ in range(3):
                        g.last = nc.tensor.transpose(PT.t[:, a_, :], listsb.t[:, a_, :], identb.t[:])
                s.op("act", lambda: A.copy(out=pmaj.t[:], in_=PT.t[:, 0:3, :]), [PT.b], [pmaj.b])
            def stageE(j, mid=None):
                pmaj = pmj[j % 2]
                for half in range(128 // TH):
                    if half == 1 and mid is not None:
                        mid()
                    ts_ = slice(half * TH, (half + 1) * TH)
                    s.op("dve", lambda: V.tensor_tensor(out=Rm.t[:], in0=io128b.t[:, None, :].to_broadcast([128, TH, 128]),
                                                        in1=pmaj.t[:, 1, ts_, None].to_broadcast([128, TH, 128]), op=ALU.is_equal),
                         [io128b.b, pmaj.b], [Rm.b])
                    s.op("pool", lambda: P.tensor_tensor(out=Rm.t[:], in0=Rm.t[:],
                                                         in1=pmaj.t[:, 2, ts_, None].to_broadcast([128, TH, 128]), op=ALU.mult),
                         [Rm.b, pmaj.b], [Rm.b])
                    s.op("dve", lambda: V.tensor_tensor(out=OH.t[:], in0=io128b.t[:, None, :].to_broadcast([128, TH, 128]),
                                                        in1=pmaj.t[:, 0, ts_, None].to_broadcast([128, TH, 128]), op=ALU.is_equal),
                         [io128b.b, pmaj.b], [OH.b])
                    for q4 in range(TH // 4):
                        pb = bank3()
                        with s.grp("pe", [Rm.b, OH.b], [pb.b]) as g:
                            for tl in range(4):
                                t_ = q4 * 4 + tl
                                g.last = nc.tensor.matmul(pb.t[:, tl, :], lhsT=Rm.t[:, t_, :], rhs=OH.t[:, t_, :], start=True, stop=True)
                        t0 = half * TH + q4 * 4
                        dst = wTt.t[:, :, t0:t0 + 4]
                        src = pb.t[:].rearrange("p t i -> p i t")
                        s.op("act", lambda: A.copy(out=dst, in_=src), [pb.b], [wTt.b])
                s.dma("sp", WT[j], wTt.t[:], reads=[wTt.b], writes=[bSALL[j]])
            if stop >= 5:
                stageQ(0)
                stageT(0)
                for j in range(NOWN):
                    if j + 1 < NOWN:
                        stageQ(j + 1)
                    stageE(j, mid=(lambda: stageT(j + 1)) if j + 1 < NOWN else None)

        NSUP = 16
        NG = 4
        s.barrier()
        with ExitStack() as p4:
            g2b = sbt(p4, "g2b", [128, D], F32)
            s.dma("sp", g2b.t[:], G12[1], reads=[bG12], writes=[g2b.b])
            ubc = [sbt(p4, "ubc%d" % i, [128, D], BF16) for i in range(2)]
            uT = sbt(p4, "uT", [128, 16, 1024], BF16)
            vb = [sbt(p4, "vb%d" % i, [128, 8, D], BF16) for i in range(2)]
            hg = [sbt(p4, "hg%d" % i, [128, 16, 512], BF16) for i in range(2)]
            wtg = [sbt(p4, "wtg%d" % i, [128, 4, 8, 128], BF16) for i in range(2)]
            gT_ = [sbt(p4, "gT%d" % i, [128, 512], BF16) for i in range(2)]
            zT = [sbt(p4, "zT%d" % i, [128, 8, 512], BF16) for i in range(2)]
            outps = [sbt(p4, "outp%d" % i, [128, D], F32) for i in range(2)]
            PUs = [pst(p4, "PU%d" % i, [128, 8, 128], BF16) for i in range(2)]
            PA = [pst(p4, "PA%d" % i, [128, 512]) for i in range(2)]
            PO = [pst(p4, "PO4%d" % i, [128, 512]) for i in range(4)]
            uview = u_tab.rearrange("(s c p) d -> s p c d", p=128, c=8)
            vview = v_tab.rearrange("(s c p) d -> s p c d", p=128, c=8)

            def load_u(sc_, c_):
                s.dma("pool", ubc[c_ % 2].t[:], uview[sc_, :, c_, :], writes=[ubc[c_ % 2].b])

            def load_v(sc_):
                for c_ in range(8):
                    s.dma("pool", vb[sc_ % 2].t[:, c_, :], vview[sc_, :, c_, :], writes=[vb[sc_ % 2].b])

            def load_group(sc_, g_, sl):
                for tl in range(4):
                    tt = g_ * 4 + tl
                    s.dma("sp", hg[sl].t[:, :, tl * 128:(tl + 1) * 128], H2T[tt], reads=[bH2T[tt]], writes=[hg[sl].b])
                    s.dma("sp", wtg[sl].t[:, tl], WT[tt, :, sc_ * 8:sc_ * 8 + 8, :], reads=[bSALL[tt]], writes=[wtg[sl].b])

            for c_ in range(2):
                load_u(0, c_)
            load_v(0)
            it = 0
            if stop >= 6:
                load_group(0, 0, 0)
            for sc in range(NSUP if stop >= 6 else 0):
                vbs = vb[sc % 2]
                for c in range(8):
                    ub_ = ubc[c % 2]
                    for kk in range(2):
                        PU = PUs[kk]
                        with s.grp("pe", [ub_.b, identb.b], [PU.b]) as g:
                            for k8 in range(8):
                                k = kk * 8 + k8
                                g.last = nc.tensor.transpose(PU.t[:, k8, :], ub_.t[:, k * 128:(k + 1) * 128], identb.t[:])
                        dst = uT.t[:, kk * 8:kk * 8 + 8, c * 128:(c + 1) * 128]
                        if (c * 2 + kk) % 2 == 0:
                            s.op("act", lambda: A.copy(out=dst, in_=PU.t[:]), [PU.b], [uT.b2])
                        else:
                            s.op("dve", lambda: V.tensor_copy(out=dst, in_=PU.t[:]), [PU.b], [uT.b])
                    if c + 2 < 8:
                        load_u(sc, c + 2)
                    elif sc + 1 < NSUP:
                        load_u(sc + 1, c - 6)
                if sc + 1 < NSUP:
                    load_v(sc + 1)
                for g_ in range(NG):
                    sl = it % 2
                    it += 1
                    if g_ + 1 < NG:
                        load_group(sc, g_ + 1, 1 - sl)
                    elif sc + 1 < NSUP:
                        load_group(sc + 1, 0, 1 - sl)
                    hgs, wts, zTs = hg[sl], wtg[sl], zT[sl]
                    for c in range(8):
                        pa = PA[c % 2]
                        with s.grp("pe", [hgs.b, uT.b, uT.b2], [pa.b]) as g:
                            for k in range(16):
                                g.last = nc.tensor.matmul(pa.t[:], lhsT=uT.t[:, k, c * 128:(c + 1) * 128], rhs=hgs.t[:, k, :],
                                                          start=(k == 0), stop=(k == 15))
                        gt = gT_[c % 2]
                        s.op("act", lambda: A.activation(out=gt.t[:], in_=pa.t[:], func=AF.Gelu), [pa.b], [gt.b])
                        s.op("dve", lambda: V.tensor_tensor(out=zTs.t[:, c, :].rearrange("p (a b) -> p a b", a=4), in0=gt.t[:].rearrange("p (a b) -> p a b", a=4),
                                                            in1=wts.t[:, :, c, :], op=ALU.mult), [gt.b, wts.b], [zTs.b])
                    for tl in range(4):
                        tt = g_ * 4 + tl
                        outp = outps[tl % 2]
                        for blk in range(4):
                            with s.grp("pe", [zTs.b, vbs.b], [PO[blk].b]) as g:
                                for c in range(8):
                                    g.last = nc.tensor.matmul(PO[blk].t[:], lhsT=zTs.t[:, c, tl * 128:(tl + 1) * 128],
                                                              rhs=vbs.t[:, c, blk * 512:(blk + 1) * 512], start=(c == 0), stop=(c == 7))
                            osl = slice(blk * 512, (blk + 1) * 512)
                            s.op("dve", lambda: V.tensor_tensor(out=outp.t[:, osl], in0=PO[blk].t[:], in1=g2b.t[:, osl], op=ALU.mult),
                                 [PO[blk].b, g2b.b], [outp.b])
                        s.dma("pool", X2[tt], outp.t[:], reads=[outp.b, outp.b2], writes=[bX2[tt]], accum_op=ALU.add)

        s.barrier()
        with ExitStack() as p5:
            fg = sbt(p5, "fg", [128, D], F32)
            s.dma("sp", fg.t[:], fin_g.to_broadcast([128, D]), writes=[fg.b])
            xf = [sbt(p5, "xf%d" % i, [128, D], F32) for i in range(2)]
            yo = [sbt(p5, "yo%d" % i, [128, D], F32) for i in range(2)]
            junk = sbt(p5, "junk5", [128, D], BF16)
            st = [sbt(p5, "st5%d" % i, [128, 2], F32) for i in range(2)]
            for tt in range(NOWN if stop >= 7 else 0):
                xx, yy, ss = xf[tt % 2], yo[tt % 2], st[tt % 2]
                s.dma("sp", xx.t[:], X2[tt], reads=[bX2[tt]], writes=[xx.b])
                s.op("act", lambda: A.activation(out=junk.t[:], in_=xx.t[:], func=AF.Square, accum_out=ss.t[:, 0:1]),
                     [xx.b], [junk.b, ss.b])
                rstd_from(None, ss.t[:, 0:1], D, [ss.b], ss, ss.t[:, 1:2])
                s.op("act", lambda: A.activation(out=yy.t[:], in_=xx.t[:], func=AF.Copy, scale=ss.t[:, 1:2]), [xx.b, ss.b], [yy.b])
                s.op("dve", lambda: V.tensor_tensor(out=yy.t[:], in0=yy.t[:], in1=fg.t[:], op=ALU.mult), [yy.b, fg.b], [yy.b])
                s.dma("sp", y_out[tt * 128:(tt + 1) * 128, :], yy.t[:], reads=[yy.b], writes=[bY[tt]])
            s.finish("sp", bY)
            if debug:
                s.finish("sp", [bROPE, bG12] + bKN + bKR + bVS + bQ + bMIXm + bMIXg + bX2 + bH2T + bSALL + bHT1)
    return nc


_NC_CACHE = {}


def _consts():
    j = np.arange(128)
    T_ = (j[:, None] <= j[None, :]).astype(np.float32)
    E_ = np.broadcast_to((j[:, None] <= 63).astype(np.float32), (128, 128))
    cm = np.stack([np.eye(128, dtype=np.float32), T_, T_ - E_, T_, 1.0 - T_], axis=1)
    return np.ascontiguousarray(cm.astype(np.float32))


def kernel(x, c, positions, ada_w, ada_b, mix_norm_g, w_in, mla_q_norm_g, mla_w_uq,
           mla_kv_norm_g, mla_w_ukv, mla_out_norm_g, gla_w_gate2, gla_b_gate,
           gla_out_norm_g, w_out, ffn_norm_g, peer_w_q, peer_sub_keys, peer_u, peer_v,
           final_norm_g):
    f = lambda a: np.ascontiguousarray(np.asarray(a, dtype=np.float32))
    x = f(x)
    positions = np.asarray(positions).astype(np.int32)
    B, S, _ = x.shape
    if "nc" not in _NC_CACHE:
        _NC_CACHE["nc"] = build_program()
    nc = _NC_CACHE["nc"]
    cm = _consts()
    col = lambda v, n: f(v).reshape(n, 128).T
    invf = (10000.0 ** (-np.arange(0, 64, 2, dtype=np.float32) / 64.0)).astype(np.float32)
    shared = {
        "cmat": cm, "ada_w": f(ada_w[0]), "ada_b": f(ada_b[0]).reshape(1, -1), "w_in": f(w_in[0]),
        "w_uq": f(mla_w_uq[0]), "w_ukv": f(mla_w_ukv[0]), "w_g2": f(gla_w_gate2[0]), "b_g": f(gla_b_gate[0]).reshape(1, -1),
        "w_out": f(w_out[0]), "w_pq": f(peer_w_q[0]), "subk": f(peer_sub_keys[0]).reshape(16, 128, 128),
        "u_tab": f(peer_u[0]), "v_tab": f(peer_v[0]), "fin_g": f(final_norm_g).reshape(1, -1),
    }
    in_maps = []
    for core in range(8):
        b, p = core // 2, core % 2
        xt = x[b].reshape(32, 128, D)
        pt = positions[b].reshape(32, 128)
        if p == 1:
            xs, ps = xt, pt
        else:
            xs = np.concatenate([np.zeros((1, 128, D), np.float32), xt[:31]], axis=0)
            ps = np.concatenate([np.zeros((1, 128), np.int32), pt[:31]], axis=0)
        vec = np.zeros((128, NVEC), np.float32)
        vec[:, C_C:C_C + 16] = col(c[b], 16)
        vec[:, C_G1:C_G1 + 16] = col(mix_norm_g[0], 16)
        vec[:, C_G2:C_G2 + 16] = col(ffn_norm_g[0], 16)
        vec[:, C_QG:C_QG + 4] = col(mla_q_norm_g[0], 4)
        vec[:, C_KVG:C_KVG + 2] = col(mla_kv_norm_g[0], 2)
        vec[:, C_MOG:C_MOG + 8] = col(mla_out_norm_g[0], 8)
        vec[:, C_GOG:C_GOG + 8] = col(gla_out_norm_g[0], 8)
        vec[0:64, C_INVF] = np.concatenate([invf, invf]) / np.float32(2 * np.pi)
        vec[0:32, C_SGN] = -1.0
        vec[32:64, C_SGN] = 1.0
        vec[:, C_FLAG] = float(p)
        vec[:, C_NEG] = (float(p) - 1.0) * 30000.0
        vec[:, C_ONE] = 1.0
        m = dict(shared)
        m["xs"] = np.ascontiguousarray(xs.reshape(NT * 128, D))
        m["pos"] = np.ascontiguousarray(ps.reshape(1, NT * 128))
        m["vec"] = vec
        in_maps.append(m)
    res = run_bass_kernel_spmd(nc, in_maps, core_ids=list(range(8)))
    out = np.empty((B, 32, 128, D), np.float32)
    for core in range(8):
        b, p = core // 2, core % 2
        y = np.asarray(res.results[core]["y_out"]).reshape(16, 128, D)
        out[b, p::2] = y
    return out.reshape(B, S, D)
```

```python
import numpy as np
import concourse.bass as bass
import concourse.mybir as mybir
from concourse.bass_utils import run_bass_kernel_spmd
from contextlib import ExitStack

F32 = mybir.dt.float32
BF16 = mybir.dt.bfloat16
I32 = mybir.dt.int32
ALU = mybir.AluOpType
AF = mybir.ActivationFunctionType
AX = mybir.AxisListType

D = 2048
NT = 32
NOWN = 16
INW = 3920
EPS = 1e-6
O_CQ, O_CKV, O_KPE, O_GQ, O_GK, O_GV, O_GLR, O_GR = 0, 512, 768, 832, 1344, 1856, 2880, 2896
C_C, C_G1, C_G2, C_QG, C_KVG, C_MOG, C_GOG, C_INVF, C_SGN, C_FLAG, C_NEG, C_ONE = 0, 16, 32, 48, 52, 54, 62, 70, 71, 72, 73, 74
NVEC = 80


DBG_TILES = 99
DBG_SUB = 99


class _StopDbg(Exception):
    pass


def _sub(n):
    if DBG_SUB <= n:
        raise _StopDbg()


class Buf:
    __slots__ = ("w", "r", "excl")

    def __init__(self, excl=False):
        self.w = None
        self.r = []
        self.excl = excl


class _Grp:
    def __init__(self, s, eng, reads, writes):
        self.s, self.eng, self.reads, self.writes = s, eng, reads, writes
        self.last = None

    def __enter__(self):
        self.s._waits(self.eng, self.reads, self.writes)
        return self

    def __exit__(self, *a):
        if a[0] is not None:
            return False
        s = self.s
        self.last.then_inc(s.sem[self.eng], 1)
        s.cnt[self.eng] += 1
        s._register((self.eng, s.cnt[self.eng]), self.reads, self.writes)
        return False


class Sched:
    def __init__(self, nc, es, n_dma_sems=32):
        self.nc = nc
        self.engs = {"pe": nc.tensor, "act": nc.scalar, "dve": nc.vector, "pool": nc.gpsimd, "sp": nc.sync}
        self.sem = {k: es.enter_context(nc.semaphore("s_" + k)) for k in self.engs}
        self.cnt = {k: 0 for k in self.engs}
        self.dsem = [es.enter_context(nc.semaphore("d_%d" % i)) for i in range(n_dma_sems)]
        self.dcnt = [0] * n_dma_sems
        self.dnext = 0
        self.dnext_sw = 0
        self.seen = {k: {} for k in self.engs}

    def _h(self, key):
        return self.sem[key] if isinstance(key, str) else self.dsem[key[1]]

    def _wait(self, eng, ev):
        key, val = ev
        if self.seen[eng].get(key, 0) >= val:
            return
        if key == eng and eng in ("pe", "sp"):
            return
        self.engs[eng].wait_ge(self._h(key), val)
        self.seen[eng][key] = val

    def _waits(self, eng, reads, writes):
        evs = {}
        for b in reads:
            if b.w is not None:
                evs[b.w[0]] = max(evs.get(b.w[0], 0), b.w[1])
        for b in writes:
            if b.w is not None:
                evs[b.w[0]] = max(evs.get(b.w[0], 0), b.w[1])
            for e in b.r:
                evs[e[0]] = max(evs.get(e[0], 0), e[1])
        for k, v in evs.items():
            self._wait(eng, (k, v))

    def _register(self, ev, reads, writes):
        for b in writes:
            b.w = ev
            b.r = []
        for b in reads:
            if b in writes:
                continue
            b.r = [e for e in b.r if e[0] != ev[0]] + [ev]

    def grp(self, eng, reads=(), writes=()):
        reads, writes = list(reads), list(writes)
        for b in reads:
            if b.excl and b not in writes:
                writes.append(b)
        return _Grp(self, eng, reads, writes)

    def op(self, eng, fn, reads=(), writes=()):
        with self.grp(eng, reads, writes) as g:
            g.last = fn()

    def dma(self, q, out, in_, reads=(), writes=(), **kw):
        reads, writes = list(reads), list(writes)
        self._waits(q, reads, writes)
        nsw = 8
        if q == "pool":
            s = self.dnext_sw
            self.dnext_sw = (s + 1) % nsw
        else:
            s = nsw + self.dnext
            self.dnext = (self.dnext + 1) % (len(self.dsem) - nsw)
        if self.dcnt[s] > 0:
            self._wait(q, (("d", s), 16 * self.dcnt[s]))
        inst = self.engs[q].dma_start(out=out, in_=in_, **kw)
        inst.then_inc(self.dsem[s], 16)
        self.dcnt[s] += 1
        self._register((("d", s), 16 * self.dcnt[s]), reads, writes)

    def barrier(self):
        for e in self.engs:
            for k in self.engs:
                if k != e and k != "sp" and self.cnt[k] > 0:
                    self._wait(e, (k, self.cnt[k]))
            for i in range(len(self.dsem)):
                if self.dcnt[i] > 0:
                    self._wait(e, (("d", i), 16 * self.dcnt[i]))

    def finish(self, eng, bufs):
        for b in bufs:
            if b.w is not None:
                self._wait(eng, b.w)


class T:
    def __init__(self, t):
        self.t = t
        self.b = Buf()
        self.b2 = Buf()


def build_program(stop=99, debug=False):
    nc = bass.Bass("TRN2", target_bir_lowering=False)
    dram_in = lambda n, sh, dt=F32: nc.dram_tensor(n, sh, dt, kind="ExternalInput").ap()
    xs = dram_in("xs", [NT * 128, D])
    pos = dram_in("pos", [1, NT * 128], I32)
    vec = dram_in("vec", [128, NVEC])
    cmat = dram_in("cmat", [128, 5, 128])
    ada_w = dram_in("ada_w", [D, 6 * D])
    ada_b = dram_in("ada_b", [1, 6 * D])
    w_in = dram_in("w_in", [D, INW])
    w_uq = dram_in("w_uq", [512, 1536])
    w_ukv = dram_in("w_ukv", [256, 2048])
    w_g2 = dram_in("w_g2", [16, 512])
    b_g = dram_in("b_g", [1, 512])
    w_out = dram_in("w_out", [D, D])
    w_pq = dram_in("w_pq", [D, D])
    subk = dram_in("subk", [16, 128, 128])
    u_tab = dram_in("u_tab", [16384, D])
    v_tab = dram_in("v_tab", [16384, D])
    fin_g = dram_in("fin_g", [1, D])
    y_out = nc.dram_tensor("y_out", [NOWN * 128, D], F32, kind="ExternalOutput").ap()

    scr = lambda n, sh, dt: (nc.dram_tensor(n, sh, dt, kind="ExternalOutput").ap() if debug else nc.dram_tensor(n, sh, dt).ap())
    ROPE = scr("rope_s", [64, 2, NT * 128], F32)
    KN = scr("kn_s", [NT, 128, 8, 128], BF16)
    KR = scr("kr_s", [NT, 64, 128], BF16)
    VS = scr("v_s", [NT, 128, 8, 128], BF16)
    QN = scr("qn_s", [NOWN, 128, 8, 128], BF16)
    QR = scr("qr_s", [NOWN, 64, 8, 128], BF16)
    MIX = scr("mix_s", [NOWN, 128, 16, 128], BF16)
    X2 = scr("x2_s", [NOWN, 128, D], F32)
    H2T = scr("h2t_s", [NOWN, 128, 16, 128], BF16)
    SALL = scr("sall_s", [NOWN, 128, 16, 128], F32)
    TK = scr("tk_s", [NOWN, 128, 16], F32)
    WT = scr("wt_s", [NOWN, 128, 128, 128], BF16)
    G12 = scr("g12_s", [2, 128, D], F32)
    HT1 = scr("ht1_s", [NOWN, 128, 16, 128], BF16)
    bHT1 = [Buf() for _ in range(NOWN)]
    bROPE, bG12 = Buf(), Buf()
    bKN = [Buf() for _ in range(NT)]
    bKR = [Buf() for _ in range(NT)]
    bVS = [Buf() for _ in range(NT)]
    bQ = [Buf() for _ in range(NOWN)]
    bMIXm = [Buf() for _ in range(NOWN)]
    bMIXg = [Buf() for _ in range(NOWN)]
    bX2 = [Buf() for _ in range(NOWN)]
    bH2T = [Buf() for _ in range(NOWN)]
    bSALL = [Buf() for _ in range(NOWN)]
    bY = [Buf() for _ in range(NOWN)]

    with ExitStack() as es:
        s = Sched(nc, es)

        def sbt(st, name, shape, dt):
            return T(st.enter_context(nc.sbuf_tensor(name, shape, dt)))

        def pst(st, name, shape, dt=F32):
            t_ = T(st.enter_context(nc.psum_tensor(name, shape, dt)))
            t_.b.excl = True
            return t_

        V, A, P = nc.vector, nc.scalar, nc.gpsimd
        fl = lambda T_: T_.t[:].rearrange("p a b -> p (a b)")
        skT = sbt(es, "skT", [128, 16, 128], BF16)

        vecs = sbt(es, "vecs", [128, NVEC], F32)
        cm = sbt(es, "cm", [128, 5, 128], F32)
        identb = sbt(es, "identb", [128, 128], BF16)
        trib = sbt(es, "trib", [128, 128], BF16)
        onesb = sbt(es, "onesb", [128, 128], BF16)
        onesf = sbt(es, "onesf", [128, 128], F32)
        AB = sbt(es, "AB", [128, 64], F32)
        epsT = sbt(es, "epsT", [128, 1], F32)
        s.dma("sp", vecs.t[:], vec, writes=[vecs.b])
        s.dma("sp", cm.t[:], cmat, writes=[cm.b])
        s.op("dve", lambda: V.tensor_copy(out=identb.t[:], in_=cm.t[:, 0, :]), [cm.b], [identb.b])
        s.op("dve", lambda: V.tensor_copy(out=trib.t[:], in_=cm.t[:, 1, :]), [cm.b], [trib.b])
        s.op("dve", lambda: V.memset(onesb.t[:], 1.0), [], [onesb.b])
        s.op("dve", lambda: V.memset(onesf.t[:], 1.0), [], [onesf.b])
        s.op("dve", lambda: V.memset(epsT.t[:], EPS), [], [epsT.b])

        def rstd_from(st_eng_dst, src_ap, n, reads, dstT, shape_ap):
            s.op("dve", lambda: V.tensor_scalar(out=shape_ap, in0=src_ap, scalar1=1.0 / n, scalar2=EPS,
                                                op0=ALU.mult, op1=ALU.add), reads, [dstT.b])
            s.op("act", lambda: A.activation(out=shape_ap, in_=shape_ap, func=AF.Sqrt), [dstT.b], [dstT.b])
            s.op("dve", lambda: V.reciprocal(out=shape_ap, in_=shape_ap), [dstT.b], [dstT.b])

        s.barrier()
        with ExitStack() as p0:
            pi = sbt(p0, "r_pi", [64, 1024], I32)
            pf = sbt(p0, "r_pf", [64, 1024], F32)
            pk = sbt(p0, "r_pk", [64, 1024], F32)
            pk2 = sbt(p0, "r_pk2", [64, 1024], F32)
            rout = sbt(p0, "r_out", [64, 2, 1024], F32)
            for ch in range(NT * 128 // 1024):
                sl = slice(ch * 1024, (ch + 1) * 1024)
                s.dma("sp", pi.t[:], pos[:, sl].to_broadcast([64, 1024]), writes=[pi.b])
                s.op("dve", lambda: V.tensor_copy(out=pf.t[:], in_=pi.t[:]), [pi.b], [pf.b])
                s.op("dve", lambda: V.tensor_scalar(out=pf.t[:], in0=pf.t[:], scalar1=vecs.t[0:64, C_INVF:C_INVF + 1],
                                                    scalar2=None, op0=ALU.mult), [pf.b, vecs.b], [pf.b])
                for which in range(2):
                    src = pf
                    if which == 0:
                        s.op("dve", lambda: V.tensor_scalar(out=pk2.t[:], in0=pf.t[:], scalar1=0.25, scalar2=None,
                                                            op0=ALU.add), [pf.b], [pk2.b])
                        src = pk2
                    else:
                        s.op("dve", lambda: V.tensor_copy(out=pk2.t[:], in_=pf.t[:]), [pf.b], [pk2.b])
                        src = pk2
                    s.op("dve", lambda: V.tensor_copy(out=pi.t[:], in_=src.t[:]), [src.b], [pi.b])
                    s.op("dve", lambda: V.tensor_copy(out=pk.t[:], in_=pi.t[:]), [pi.b], [pk.b])
                    s.op("dve", lambda: V.tensor_tensor(out=src.t[:], in0=src.t[:], in1=pk.t[:], op=ALU.subtract),
                         [src.b, pk.b], [src.b])
                    s.op("dve", lambda: V.tensor_scalar(out=pk.t[:], in0=src.t[:], scalar1=0.5, scalar2=None,
                                                        op0=ALU.is_ge), [src.b], [pk.b])
                    s.op("dve", lambda: V.tensor_tensor(out=src.t[:], in0=src.t[:], in1=pk.t[:], op=ALU.subtract),
                         [src.b, pk.b], [src.b])
                    s.op("dve", lambda: V.tensor_scalar(out=pk.t[:], in0=src.t[:], scalar1=-0.5, scalar2=None,
                                                        op0=ALU.is_lt), [src.b], [pk.b])
                    s.op("dve", lambda: V.tensor_tensor(out=src.t[:], in0=src.t[:], in1=pk.t[:], op=ALU.add),
                         [src.b, pk.b], [src.b])
                    s.op("act", lambda: A.activation(out=rout.t[:, which, :], in_=src.t[:], func=AF.Sin,
                                                     scale=float(2 * np.pi)), [src.b], [rout.b])
                s.op("dve", lambda: V.tensor_scalar(out=rout.t[:, 1, :], in0=rout.t[:, 1, :],
                                                    scalar1=vecs.t[0:64, C_SGN:C_SGN + 1], scalar2=None, op0=ALU.mult),
                     [rout.b, vecs.b], [rout.b])
                s.dma("sp", ROPE[:, :, sl], rout.t[:], reads=[rout.b], writes=[bROPE])

        s.barrier()
        with ExitStack() as p0:
            cact = sbt(p0, "cact", [128, 16], F32)
            adab = sbt(p0, "adab", [1, 6 * D], F32)
            modrow = sbt(p0, "modrow", [1, 6 * D], F32)
            wblk = [sbt(p0, "wblk%d" % i, [128, 16, 512], F32) for i in range(2)]
            pm = [pst(p0, "pm%d" % i, [128, 512]) for i in range(2)]
            pcol = pst(p0, "pcol", [128, 64])
            cols = sbt(p0, "cols", [128, 64], F32)
            gb = sbt(p0, "gb", [128, D], F32)
            s.dma("sp", adab.t[:], ada_b, writes=[adab.b])
            s.op("act", lambda: A.activation(out=cact.t[:], in_=vecs.t[:, C_C:C_C + 16], func=AF.Silu),
                 [vecs.b], [cact.b])
            adv = ada_w.rearrange("(k p) n -> p k n", p=128)
            for blk in range(24):
                wb = wblk[blk % 2]
                pp = pm[blk % 2]
                s.dma("sp", wb.t[:], adv[:, :, blk * 512:(blk + 1) * 512], writes=[wb.b])
                with s.grp("pe", [wb.b, cact.b], [pp.b]) as g:
                    for k in range(16):
                        g.last = nc.tensor.matmul(pp.t[0:1, :], lhsT=cact.t[:, k:k + 1], rhs=wb.t[:, k, :],
                                                  start=(k == 0), stop=(k == 15))
                s.op("dve", lambda: V.tensor_tensor(out=modrow.t[0:1, blk * 512:(blk + 1) * 512], in0=pp.t[0:1, :],
                                                    in1=adab.t[0:1, blk * 512:(blk + 1) * 512], op=ALU.add),
                     [pp.b, adab.b], [modrow.b])
            with s.grp("pe", [modrow.b, onesf.b], [pcol.b]) as g:
                for vi, v in enumerate((0, 1, 3, 4)):
                    for k in range(16):
                        off = v * D + k * 128
                        g.last = nc.tensor.matmul(pcol.t[:, vi * 16 + k:vi * 16 + k + 1],
                                                  lhsT=modrow.t[0:1, off:off + 128], rhs=onesf.t[0:1, 0:1],
                                                  start=True, stop=True)
            s.op("dve", lambda: V.tensor_copy(out=cols.t[:], in_=pcol.t[:]), [pcol.b], [cols.b])
            for (dst, gcol, sc, sh) in ((0, C_G1, 16, 0), (32, C_G2, 48, 32)):
                s.op("dve", lambda: V.scalar_tensor_tensor(out=AB.t[:, dst:dst + 16], in0=cols.t[:, sc:sc + 16], scalar=1.0,
                                                           in1=vecs.t[:, gcol:gcol + 16], op0=ALU.add, op1=ALU.mult),
                     [cols.b, vecs.b], [AB.b])
                s.op("dve", lambda: V.tensor_copy(out=AB.t[:, dst + 16:dst + 32], in_=cols.t[:, sh:sh + 16]),
                     [cols.b], [AB.b])
            for gi, v in enumerate((2, 5)):
                for j in range(4):
                    pp = pm[j % 2]
                    s.op("pe", lambda: nc.tensor.matmul(pp.t[:], lhsT=onesf.t[0:1, :],
                                                        rhs=modrow.t[0:1, v * D + j * 512:v * D + (j + 1) * 512],
                                                        start=True, stop=True), [modrow.b, onesf.b], [pp.b])
                    s.op("dve", lambda: V.tensor_copy(out=gb.t[:, j * 512:(j + 1) * 512], in_=pp.t[:]), [pp.b], [gb.b])
                s.dma("sp", G12[gi], gb.t[:], reads=[gb.b], writes=[bG12])

        s.barrier()
        with ExitStack() as p1:
            win = sbt(p1, "win", [128, 16, O_GR], BF16)
            kpesw = sbt(p1, "kpesw", [128, 16, 64], BF16)
            wuq = sbt(p1, "wuq", [128, 4, 1536], BF16)
            wqsw = sbt(p1, "wqsw", [128, 4, 8, 64], BF16)
            wukv = sbt(p1, "wukv", [128, 2, 2048], BF16)
            w2f = sbt(p1, "w2f", [33, 512], F32)
            w2a = sbt(p1, "w2a", [33, 512], BF16)
            glrT = sbt(p1, "glrT", [33, 128], BF16)
            Sst = sbt(p1, "Sst", [128, 4, 256], F32)
            Sbf = sbt(p1, "Sbf", [128, 4, 256], BF16)
            w_in_v = w_in.rearrange("(k p) n -> p k n", p=128)
            winb = {}
            for nm_, a_, b_ in (("ckv", O_CKV, O_GQ), ("glr", O_GLR, O_GR), ("gk", O_GK, O_GV), ("gv", O_GV, O_GLR),
                                ("cq", O_CQ, O_CKV), ("gq", O_GQ, O_GK)):
                winb[nm_] = Buf()
                step_ = 512
                for c0_ in range(a_, b_, step_):
                    c1_ = min(b_, c0_ + step_)
                    s.dma("pool", win.t[:, :, c0_:c1_], w_in_v[:, :, c0_:c1_], writes=[winb[nm_]])
            s.dma("pool", wuq.t[:], w_uq.rearrange("(k p) n -> p k n", p=128), writes=[wuq.b])
            s.dma("pool", wukv.t[:], w_ukv.rearrange("(k p) n -> p k n", p=128), writes=[wukv.b])
            s.op("dve", lambda: V.memset(w2f.t[:], 0.0), [], [w2f.b])
            s.dma("sp", w2f.t[0:16, :], w_g2, writes=[w2f.b])
            s.dma("sp", w2f.t[32:33, :], b_g, writes=[w2f.b])
            s.op("dve", lambda: V.tensor_copy(out=w2a.t[:], in_=w2f.t[:]), [w2f.b], [w2a.b])
            s.op("dve", lambda: V.memset(glrT.t[:], 0.0), [], [glrT.b])
            s.op("dve", lambda: V.memset(glrT.t[32:33, :], 1.0), [glrT.b], [glrT.b])
            s.op("dve", lambda: V.memset(Sst.t[:], 0.0), [], [Sst.b])
            s.op("dve", lambda: V.memset(Sbf.t[:], 0.0), [], [Sbf.b])
            for k in range(4):
                s.op("dve", lambda: V.tensor_scalar(out=wuq.t[:, k, :], in0=wuq.t[:, k, :],
                                                    scalar1=vecs.t[:, C_QG + k:C_QG + k + 1], scalar2=None, op0=ALU.mult),
                     [wuq.b, vecs.b], [wuq.b])
            for k in range(2):
                s.op("dve", lambda: V.tensor_scalar(out=wukv.t[:, k, :], in0=wukv.t[:, k, :],
                                                    scalar1=vecs.t[:, C_KVG + k:C_KVG + k + 1], scalar2=None, op0=ALU.mult),
                     [wukv.b, vecs.b], [wukv.b])
            wuq4 = wuq.t[:].rearrange("p k (h c) -> p k h c", c=192)
            s.op("dve", lambda: V.tensor_copy(out=wqsw.t[:, :, :, 0:32], in_=wuq4[:, :, :, 160:192]), [wuq.b], [wqsw.b])
            s.op("dve", lambda: V.tensor_copy(out=wqsw.t[:, :, :, 32:64], in_=wuq4[:, :, :, 128:160]), [wqsw.b, wuq.b], [wqsw.b])
            s.op("dve", lambda: V.tensor_copy(out=kpesw.t[:, :, 0:32], in_=win.t[:, :, O_KPE + 32:O_KPE + 64]), [winb["ckv"]], [kpesw.b])
            s.op("dve", lambda: V.tensor_copy(out=kpesw.t[:, :, 32:64], in_=win.t[:, :, O_KPE:O_KPE + 32]), [kpesw.b, winb["ckv"]], [kpesw.b])

            xt = [sbt(p1, "xt%d" % i, [128, D], F32) for i in range(1)]
            rt = [sbt(p1, "rt%d" % i, [64, 2, 128], F32) for i in range(3)]
            xns = [sbt(p1, "xn%d" % i, [128, D], BF16) for i in range(2)]
            hTs = [sbt(p1, "hT%d" % i, [128, 16, 128], BF16) for i in range(2)]
            hT = hTs[0]
            st1s = [sbt(p1, "st1%d" % i, [128, 4], F32) for i in range(2)]
            latr = sbt(p1, "latr", [128, 4, 128], F32)
            latsq = sbt(p1, "latsq", [128, 4, 128], BF16)
            latn = sbt(p1, "latn", [128, 4, 128], BF16)
            rsb = sbt(p1, "rsb", [128, 4, 128], F32)
            kNs = sbt(p1, "kNs", [128, 8, 128], BF16)
            vs = sbt(p1, "vs", [128, 8, 128], BF16)
            rp1 = sbt(p1, "rp1", [64, 4, 128], F32)
            rp2 = sbt(p1, "rp2", [64, 4, 128], F32)
            kRs = sbt(p1, "kRs", [64, 128], BF16)
            qNs = sbt(p1, "qNs", [128, 8, 128], BF16)
            qRs = sbt(p1, "qRs", [64, 8, 128], BF16)
            la = sbt(p1, "la", [128, 512], F32)
            gktm = sbt(p1, "gktm", [128, 512], F32)
            gvb = sbt(p1, "gvb", [128, 1024], BF16)
            e1 = sbt(p1, "e1", [128, 4, 128], BF16)
            e2 = sbt(p1, "e2", [128, 4, 128], BF16)
            e4 = sbt(p1, "e4", [128, 4, 128], BF16)
            e3 = sbt(p1, "e3", [128, 512], BF16)
            dec = sbt(p1, "dec", [128, 4], F32)
            qeT = sbt(p1, "qeT", [128, 4, 128], BF16)
            qbT = sbt(p1, "qbT", [128, 4, 128], BF16)
            keT = sbt(p1, "keT", [128, 4, 128], BF16)
            ke2 = sbt(p1, "ke2", [128, 512], BF16)
            ATm = sbt(p1, "ATm", [128, 4, 128], BF16)
            ogr = sbt(p1, "ogr", [128, 8, 128], F32)
            ogsq = sbt(p1, "ogsq", [128, 8, 128], BF16)
            mixg = sbt(p1, "mixg", [128, 8, 128], BF16)
            PT = pst(p1, "PT", [128, 16, 128], BF16)
            PB = [pst(p1, "PB%d" % i, [128, 4, 128]) for i in range(6)]
            pbi = [0]

            def bank():
                b = PB[pbi[0] % 6]
                pbi[0] += 1
                return b

            def load_tile(i):
                s.dma("sp", xt[0].t[:], xs[i * 128:(i + 1) * 128, :], writes=[xt[0].b])
                s.dma("sp", rt[i % 3].t[:], ROPE[:, :, i * 128:(i + 1) * 128], reads=[bROPE], writes=[rt[i % 3].b])

            def norm_front(i_):
                xtile, xn, st1 = xt[0], xns[i_ % 2], st1s[i_ % 2]
                s.op("act", lambda: A.activation(out=xn.t[:], in_=xtile.t[:], func=AF.Square, accum_out=st1.t[:, 0:1]),
                     [xtile.b], [xn.b, st1.b])
                rstd_from(None, st1.t[:, 0:1], D, [st1.b], st1, st1.t[:, 1:2])
                s.op("act", lambda: A.activation(out=xn.t[:], in_=xtile.t[:], func=AF.Copy, scale=st1.t[:, 1:2]),
                     [xtile.b, st1.b], [xn.b])
                if i_ + 1 < NT:
                    load_tile(i_ + 1)

            def norm_back(i_, abcol, hdst):
                xn = xns[i_ % 2]
                with s.grp("pe", [xn.b, identb.b], [PT.b]) as g:
                    for k in range(16):
                        g.last = nc.tensor.transpose(PT.t[:, k, :], xn.t[:, k * 128:(k + 1) * 128], identb.t[:])
                for k in range(16):
                    if k % 2 == 0:
                        s.op("dve", lambda: V.tensor_scalar(out=hdst.t[:, k, :], in0=PT.t[:, k, :],
                                                            scalar1=AB.t[:, abcol + k:abcol + k + 1],
                                                            scalar2=AB.t[:, abcol + 16 + k:abcol + 17 + k],
                                                            op0=ALU.mult, op1=ALU.add), [PT.b, AB.b], [hdst.b])
                    else:
                        s.op("act", lambda: A.activation(out=hdst.t[:, k, :], in_=PT.t[:, k, :], func=AF.Identity,
                                                         scale=AB.t[:, abcol + k:abcol + k + 1],
                                                         bias=AB.t[:, abcol + 16 + k:abcol + 17 + k]), [PT.b, AB.b], [hdst.b2])

            def proj_fm(pb, slot, col0, m, rows=None, wt=None):
                wt = wt if wt is not None else win
                for k in range(16):
                    lhsT = wt.t[:, k, col0:col0 + m]
                    last = nc.tensor.matmul(pb.t[0:m, slot, :], lhsT=lhsT, rhs=hT.t[:, k, :], start=(k == 0), stop=(k == 15))
                return last

            def latent_norm(pb, nch, n):
                s.op("act", lambda: A.activation(out=latsq.t[:, 0:nch, :], in_=pb.t[:, 0:nch, :], func=AF.Square), [pb.b], [latsq.b])
                s.op("dve", lambda: V.tensor_copy(out=latr.t[:, 0:nch, :], in_=pb.t[:, 0:nch, :]), [pb.b], [latr.b])
                _sub(2.2)
                pr = bank()
                with s.grp("pe", [latsq.b, onesb.b], [pr.b]) as g:
                    for c in range(nch):
                        g.last = nc.tensor.matmul(pr.t[:, 0, :], lhsT=onesb.t[:], rhs=latsq.t[:, c, :], start=(c == 0), stop=(c == nch - 1))
                _sub(2.4)
                rstd_from(None, pr.t[:, 0, :], n, [pr.b], rsb, rsb.t[:, 0, :])
                _sub(2.6)
                s.op("dve", lambda: V.tensor_tensor(out=latn.t[:, 0:nch, :], in0=latr.t[:, 0:nch, :],
                                                    in1=rsb.t[:, 0:1, :].to_broadcast([128, nch, 128]), op=ALU.mult),
                     [latr.b, rsb.b], [latn.b])

            load_tile(0)
            try:
                for i in range(min(NT if stop >= 1 else 0, DBG_TILES)):
                    own = (i % 2 == 1)
                    j = i // 2
                    r_i = rt[i % 3]
                    hT = hTs[i % 2]
                    if i == 0:
                        norm_front(0)
                        norm_back(0, 0, hT)
                    _sub(1)
                    if own:
                        s.dma("sp", HT1[j], hT.t[:], reads=[hT.b, hT.b2], writes=[bHT1[j]])
                    pb = bank()
                    with s.grp("pe", [hT.b, hT.b2, winb["ckv"], kpesw.b], [pb.b]) as g:
                        proj_fm(pb, 0, O_CKV, 128)
                        proj_fm(pb, 1, O_CKV + 128, 128)
                        proj_fm(pb, 2, O_KPE, 64)
                        g.last = proj_fm(pb, 3, 0, 64, wt=kpesw)
                    s.op("dve", lambda: V.tensor_tensor(out=rp1.t[:, 0, :], in0=pb.t[0:64, 2, :], in1=r_i.t[:, 0, :], op=ALU.mult),
                         [pb.b, r_i.b], [rp1.b])
                    s.op("dve", lambda: V.tensor_tensor(out=rp2.t[:, 0, :], in0=pb.t[0:64, 3, :], in1=r_i.t[:, 1, :], op=ALU.mult),
                         [pb.b, r_i.b], [rp2.b])
                    s.op("pool", lambda: P.tensor_tensor(out=kRs.t[:], in0=rp1.t[:, 0, :], in1=rp2.t[:, 0, :], op=ALU.add),
                         [rp1.b, rp2.b], [kRs.b])
                    s.dma("sp", KR[i], kRs.t[:], reads=[kRs.b], writes=[bKR[i]])
                    _sub(2)
                    latent_norm(pb, 2, 256)
                    _sub(3)
                    pk0, pk1 = bank(), bank()
                    with s.grp("pe", [latn.b, wukv.b], [pk0.b, pk1.b]) as g:
                        for h in range(8):
                            pk = pk0 if h < 4 else pk1
                            for c in range(2):
                                g.last = nc.tensor.matmul(pk.t[:, h % 4, :], lhsT=wukv.t[:, c, h * 256:h * 256 + 128],
                                                          rhs=latn.t[:, c, :], start=(c == 0), stop=(c == 1))
                    s.op("act", lambda: A.copy(out=kNs.t[:, 0:4, :], in_=pk0.t[:]), [pk0.b], [kNs.b])
                    s.op("act", lambda: A.copy(out=kNs.t[:, 4:8, :], in_=pk1.t[:]), [pk1.b, kNs.b], [kNs.b])
                    s.dma("sp", KN[i], kNs.t[:], reads=[kNs.b], writes=[bKN[i]])
                    _sub(4)
                    pv0, pv1 = bank(), bank()
                    wv4 = wukv.t[:].rearrange("p k (h c) -> p k h c", c=256)
                    with s.grp("pe", [latn.b, wukv.b], [pv0.b, pv1.b]) as g:
                        for hb in range(2):
                            pv = pv0 if hb == 0 else pv1
                            for c in range(2):
                                g.last = nc.tensor.matmul(pv.t[:], lhsT=latn.t[:, c, :], rhs=wv4[:, c, hb * 4:hb * 4 + 4, 128:256],
                                                          start=(c == 0), stop=(c == 1))
                    s.op("dve", lambda: V.tensor_copy(out=vs.t[:, 0:4, :], in_=pv0.t[:]), [pv0.b], [vs.b])
                    s.op("dve", lambda: V.tensor_copy(out=vs.t[:, 4:8, :], in_=pv1.t[:]), [pv1.b, vs.b], [vs.b])
                    s.dma("sp", VS[i], vs.t[:], reads=[vs.b], writes=[bVS[i]])
                    if i + 1 < NT:
                        norm_front(i + 1)
                        norm_back(i + 1, 0, hTs[(i + 1) % 2])
                    _sub(5)
                    if own:
                        pb = bank()
                        with s.grp("pe", [hT.b, hT.b2, winb["cq"]], [pb.b]) as g:
                            for c in range(4):
                                g.last = proj_fm(pb, c, O_CQ + c * 128, 128)
                        latent_norm(pb, 4, 512)
                        pq0, pq1 = bank(), bank()
                        with s.grp("pe", [latn.b, wuq.b], [pq0.b, pq1.b]) as g:
                            for h in range(8):
                                pq = pq0 if h < 4 else pq1
                                for c in range(4):
                                    g.last = nc.tensor.matmul(pq.t[:, h % 4, :], lhsT=wuq.t[:, c, h * 192:h * 192 + 128],
                                                              rhs=latn.t[:, c, :], start=(c == 0), stop=(c == 3))
                        s.op("act", lambda: A.copy(out=qNs.t[:, 0:4, :], in_=pq0.t[:]), [pq0.b], [qNs.b])
                        s.op("act", lambda: A.copy(out=qNs.t[:, 4:8, :], in_=pq1.t[:]), [pq1.b, qNs.b], [qNs.b])
                        for hb in range(2):
                            pa, pbb = bank(), bank()
                            with s.grp("pe", [latn.b, wuq.b, wqsw.b], [pa.b, pbb.b]) as g:
                                for hh in range(4):
                                    h = hb * 4 + hh
                                    for c in range(4):
                                        nc.tensor.matmul(pa.t[0:64, hh, :], lhsT=wuq.t[:, c, h * 192 + 128:h * 192 + 192],
                                                         rhs=latn.t[:, c, :], start=(c == 0), stop=(c == 3))
                                    for c in range(4):
                                        g.last = nc.tensor.matmul(pbb.t[0:64, hh, :], lhsT=wqsw.t[:, c, h, :],
                                                                  rhs=latn.t[:, c, :], start=(c == 0), stop=(c == 3))
                            hs = slice(hb * 4, hb * 4 + 4)
                            s.op("dve", lambda: V.tensor_tensor(out=rp1.t[:], in0=pa.t[0:64, :, :],
                                                                in1=r_i.t[:, 0:1, :].to_broadcast([64, 4, 128]), op=ALU.mult),
                                 [pa.b, r_i.b], [rp1.b])
                            s.op("dve", lambda: V.tensor_tensor(out=rp2.t[:], in0=pbb.t[0:64, :, :],
                                                                in1=r_i.t[:, 1:2, :].to_broadcast([64, 4, 128]), op=ALU.mult),
                                 [pbb.b, r_i.b], [rp2.b])
                            s.op("pool", lambda: P.tensor_tensor(out=qRs.t[:, hs, :], in0=rp1.t[:], in1=rp2.t[:], op=ALU.add),
                                 [rp1.b, rp2.b], [qRs.b])
                        s.dma("sp", QN[j], qNs.t[:], reads=[qNs.b], writes=[bQ[j]])
                        s.dma("sp", QR[j], qRs.t[:], reads=[qRs.b], writes=[bQ[j]])
                    pg = bank()
                    s.op("pe", lambda: proj_fm(pg, 0, O_GLR, 16), [hT.b, hT.b2, winb["glr"]], [pg.b])
                    s.op("act", lambda: A.copy(out=glrT.t[0:16, :], in_=pg.t[0:16, 0, :]), [pg.b], [glrT.b])
                    ppre = bank()
                    s.op("pe", lambda: nc.tensor.matmul(fl(ppre), lhsT=glrT.t[:], rhs=w2a.t[:], start=True, stop=True),
                         [glrT.b, w2a.b], [ppre.b])
                    s.op("act", lambda: A.activation(out=la.t[:], in_=ppre.t[:].rearrange("p a b -> p (a b)"), func=AF.Exp, scale=-1.0),
                         [ppre.b], [la.b])
                    s.op("act", lambda: A.activation(out=la.t[:], in_=la.t[:], func=AF.Ln, bias=1.0), [la.b], [la.b])
                    fcol = C_FLAG if i == 0 else C_ONE
                    s.op("dve", lambda: V.tensor_scalar(out=la.t[:], in0=la.t[:], scalar1=-1.0 / 16.0,
                                                        scalar2=vecs.t[:, fcol:fcol + 1], op0=ALU.mult, op1=ALU.mult),
                         [la.b, vecs.b], [la.b])
                    pgk = bank()
                    with s.grp("pe", [hT.b, hT.b2, winb["gk"]], [pgk.b]) as g:
                        for k in range(16):
                            g.last = nc.tensor.matmul(fl(pgk), lhsT=hT.t[:, k, :], rhs=win.t[:, k, O_GK:O_GK + 512],
                                                      start=(k == 0), stop=(k == 15))
                    s.op("act", lambda: A.copy(out=gktm.t[:], in_=pgk.t[:].rearrange("p a b -> p (a b)")), [pgk.b], [gktm.b])
                    pgv0, pgv1 = bank(), bank()
                    with s.grp("pe", [hT.b, hT.b2, winb["gv"]], [pgv0.b, pgv1.b]) as g:
                        for hb in range(2):
                            pv = pgv0 if hb == 0 else pgv1
                            for k in range(16):
                                g.last = nc.tensor.matmul(fl(pv), lhsT=hT.t[:, k, :],
                                                          rhs=win.t[:, k, O_GV + hb * 512:O_GV + (hb + 1) * 512],
                                                          start=(k == 0), stop=(k == 15))
                    s.op("dve", lambda: V.tensor_copy(out=gvb.t[:, 0:512], in_=pgv0.t[:].rearrange("p a b -> p (a b)")), [pgv0.b], [gvb.b])
                    s.op("dve", lambda: V.tensor_copy(out=gvb.t[:, 512:1024], in_=pgv1.t[:].rearrange("p a b -> p (a b)")), [pgv1.b, gvb.b], [gvb.b])
                    pr3 = bank()
                    s.op("pe", lambda: nc.tensor.matmul(fl(pr3), lhsT=cm.t[:, 4, :], rhs=la.t[:], start=True, stop=True),
                         [cm.b, la.b], [pr3.b])
                    s.op("act", lambda: A.activation(out=e3.t[:], in_=pr3.t[:].rearrange("p a b -> p (a b)"), func=AF.Exp), [pr3.b], [e3.b])
                    pbl = bank()
                    with s.grp("pe", [la.b, onesf.b], [pbl.b]) as g:
                        for h in range(4):
                            g.last = nc.tensor.matmul(pbl.t[:, 0, h:h + 1], lhsT=la.t[:, h * 128:(h + 1) * 128], rhs=onesf.t[:, 0:1],
                                                      start=True, stop=True)
                    s.op("act", lambda: A.activation(out=dec.t[:], in_=pbl.t[:, 0, 0:4], func=AF.Exp), [pbl.b], [dec.b])
                    s.op("dve", lambda: V.scalar_tensor_tensor(out=ke2.t[:], in0=gktm.t[:], scalar=vecs.t[:, fcol:fcol + 1],
                                                               in1=e3.t[:], op0=ALU.mult, op1=ALU.mult),
                         [gktm.b, e3.b, vecs.b], [ke2.b])
                    if own:
                        pgq = bank()
                        with s.grp("pe", [hT.b, hT.b2, winb["gq"]], [pgq.b]) as g:
                            for h in range(4):
                                g.last = proj_fm(pgq, h, O_GQ + h * 128, 128)
                        pgkT = bank()
                        with s.grp("pe", [hT.b, hT.b2, winb["gk"]], [pgkT.b]) as g:
                            for h in range(4):
                                g.last = proj_fm(pgkT, h, O_GK + h * 128, 128)
                        pc1, pbc = bank(), bank()
                        with s.grp("pe", [la.b, cm.b], [pc1.b, pbc.b]) as g:
                            for h in range(4):
                                nc.tensor.matmul(pc1.t[:, h, :], lhsT=la.t[:, h * 128:(h + 1) * 128], rhs=cm.t[:, 2, :], start=True, stop=True)
                                g.last = nc.tensor.matmul(pbc.t[:, h, :], lhsT=la.t[:, h * 128:(h + 1) * 128], rhs=cm.t[:, 3, :], start=True, stop=True)
                        s.op("act", lambda: A.activation(out=e1.t[:], in_=pc1.t[:], func=AF.Exp), [pc1.b], [e1.b])
                        s.op("act", lambda: A.activation(out=e2.t[:], in_=pc1.t[:], func=AF.Exp, scale=-1.0), [pc1.b], [e2.b])
                        s.op("act", lambda: A.activation(out=e4.t[:], in_=pbc.t[:], func=AF.Exp), [pbc.b], [e4.b])
                        sc = 128.0 ** -0.5
                        s.op("dve", lambda: V.scalar_tensor_tensor(out=qeT.t[:], in0=pgq.t[:], scalar=sc, in1=e1.t[:],
                                                                   op0=ALU.mult, op1=ALU.mult), [pgq.b, e1.b], [qeT.b])
                        s.op("dve", lambda: V.scalar_tensor_tensor(out=qbT.t[:], in0=pgq.t[:], scalar=sc, in1=e4.t[:],
                                                                   op0=ALU.mult, op1=ALU.mult), [pgq.b, e4.b], [qbT.b])
                        s.op("dve", lambda: V.tensor_tensor(out=keT.t[:], in0=pgkT.t[:], in1=e2.t[:], op=ALU.mult),
                             [pgkT.b, e2.b], [keT.b])
                        pat = bank()
                        with s.grp("pe", [keT.b, qeT.b], [pat.b]) as g:
                            for h in range(4):
                                g.last = nc.tensor.matmul(pat.t[:, h, :], lhsT=keT.t[:, h, :], rhs=qeT.t[:, h, :], start=True, stop=True)
                        s.op("dve", lambda: V.tensor_tensor(out=ATm.t[:], in0=pat.t[:],
                                                            in1=cm.t[:, 1:2, :].to_broadcast([128, 4, 128]), op=ALU.mult),
                             [pat.b, cm.b], [ATm.b])
                        po0, po1 = bank(), bank()
                        with s.grp("pe", [Sbf.b, qbT.b, gvb.b, ATm.b], [po0.b, po1.b]) as g:
                            for h in range(4):
                                for c in range(2):
                                    po = po0 if h < 2 else po1
                                    slot = (h % 2) * 2 + c
                                    nc.tensor.matmul(po.t[:, slot, :], lhsT=Sbf.t[:, h, c * 128:(c + 1) * 128], rhs=qbT.t[:, h, :],
                                                     start=True, stop=False)
                                    g.last = nc.tensor.matmul(po.t[:, slot, :], lhsT=gvb.t[:, h * 256 + c * 128:h * 256 + (c + 1) * 128],
                                                              rhs=ATm.t[:, h, :], start=False, stop=True)
                        for hb, po in enumerate((po0, po1)):
                            s.op("act", lambda: A.activation(out=ogsq.t[:, hb * 4:hb * 4 + 4, :], in_=po.t[:], func=AF.Square), [po.b], [ogsq.b])
                            s.op("dve", lambda: V.tensor_copy(out=ogr.t[:, hb * 4:hb * 4 + 4, :], in_=po.t[:]), [po.b], [ogr.b])
                        pss = bank()
                        with s.grp("pe", [ogsq.b, onesb.b], [pss.b]) as g:
                            for h in range(4):
                                for c in range(2):
                                    g.last = nc.tensor.matmul(pss.t[:, h, :], lhsT=onesb.t[:], rhs=ogsq.t[:, h * 2 + c, :],
                                                              start=(c == 0), stop=(c == 1))
                        rstd_from(None, pss.t[:], 256, [pss.b], rsb, rsb.t[:])
                        og4 = ogr.t[:].rearrange("p (h c) t -> p h c t", c=2)
                        s.op("dve", lambda: V.tensor_tensor(out=og4, in0=og4, in1=rsb.t[:, :, None, :].to_broadcast([128, 4, 2, 128]),
                                                            op=ALU.mult), [ogr.b, rsb.b], [ogr.b])
                        s.op("pool", lambda: P.tensor_tensor(out=mixg.t[:], in0=ogr.t[:],
                                                             in1=vecs.t[:, C_GOG:C_GOG + 8, None].to_broadcast([128, 8, 128]),
                                                             op=ALU.mult), [ogr.b, vecs.b], [mixg.b])
                        s.dma("sp", MIX[j, :, 8:16, :], mixg.t[:], reads=[mixg.b], writes=[bMIXg[j]])
                    pu0, pu1 = bank(), bank()
                    with s.grp("pe", [ke2.b, gvb.b], [pu0.b, pu1.b]) as g:
                        for h in range(4):
                            pu = pu0 if h < 2 else pu1
                            g.last = nc.tensor.matmul(pu.t[:, (h % 2) * 2:(h % 2) * 2 + 2, :].rearrange("p a b -> p (a b)"), lhsT=ke2.t[:, h * 128:(h + 1) * 128],
                                                      rhs=gvb.t[:, h * 256:(h + 1) * 256], start=True, stop=True)
                    for h in range(4):
                        pu = pu0 if h < 2 else pu1
                        s.op("dve", lambda: V.scalar_tensor_tensor(out=Sst.t[:, h, :], in0=Sst.t[:, h, :], scalar=dec.t[:, h:h + 1],
                                                                   in1=pu.t[:, (h % 2) * 2:(h % 2) * 2 + 2, :].rearrange("p a b -> p (a b)"),
                                                                   op0=ALU.mult, op1=ALU.add), [Sst.b, dec.b, pu.b], [Sst.b])
                    s.op("act", lambda: A.copy(out=Sbf.t[:], in_=Sst.t[:]), [Sst.b], [Sbf.b])
            except _StopDbg:
                pass

        s.barrier()
        with ExitStack() as p2:
            KNs = sbt(p2, "KNs", [128, NT, 8, 128], BF16)
            VSs = sbt(p2, "VSs", [128, NT, 8, 128], BF16)
            KRs = sbt(p2, "KRs", [128, NT, 128], BF16)
            bk = [Buf() for _ in range(NT)]
            s.op("pool", lambda: P.memset(KRs.t[64:128, :, :], 0.0), [], [KRs.b])
            bkN = [Buf() for _ in range(NT)]
            bkV = [Buf() for _ in range(NT)]
            for i0, i1 in ((0, 2), (2, 8), (8, 16), (16, 24), (24, 32)):
                grp_ = list(range(i0, i1))
                s.dma("sp", KNs.t[:, i0:i1], KN[i0:i1].rearrange("i p h t -> p i h t"), reads=[bKN[i] for i in grp_], writes=[bkN[i] for i in grp_])
                s.dma("sp", KRs.t[0:64, i0:i1, :], KR[i0:i1].rearrange("i p t -> p i t"), reads=[bKR[i] for i in grp_] + [KRs.b], writes=[bk[i] for i in grp_])
                s.dma("sp", VSs.t[:, i0:i1], VS[i0:i1].rearrange("i p h t -> p i h t"), reads=[bVS[i] for i in grp_], writes=[bkV[i] for i in grp_])
            PS = [[pst(p2, "PS%d%d" % (a_, b_), [128, 4, 128]) for b_ in range(2)] for a_ in range(2)]
            PO = [pst(p2, "PO%d" % b_, [128, 4, 128]) for b_ in range(2)]
            PL = [pst(p2, "PL%d" % b_, [128, 4, 128]) for b_ in range(2)]
            qn = [sbt(p2, "qn%d" % i, [128, 8, 128], BF16) for i in range(2)]
            qr = [sbt(p2, "qr%d" % i, [128, 8, 128], BF16) for i in range(2)]
            pT = [sbt(p2, "pT%d" % i, [128, 8, 128], BF16) for i in range(2)]
            Lacc = sbt(p2, "Lacc", [128, 8, 128], F32)
            Lr = sbt(p2, "Lr", [128, 8, 128], F32)
            of = sbt(p2, "of", [128, 8, 128], F32)
            osq = sbt(p2, "osq", [128, 8, 128], BF16)
            rs8 = sbt(p2, "rs8", [128, 8, 128], F32)
            mixm = sbt(p2, "mixm", [128, 8, 128], BF16)
            for i in range(2):
                s.op("pool", lambda: P.memset(qr[i].t[64:128, :, :], 0.0), [], [qr[i].b])
            scale = 192.0 ** -0.5
            LaccD = sbt(p2, "LaccD", [128, 8, 128], F32)
            pairs = [(j, kt) for j in range(NOWN if stop >= 3 else 0) for kt in range(2 * j + 2)]

            def emit_S(n):
                j, kt = pairs[n]
                qn_j, qr_j = qn[j % 2], qr[j % 2]
                if kt == 0:
                    s.dma("sp", qn_j.t[:], QN[j], reads=[bQ[j]], writes=[qn_j.b])
                    s.dma("sp", qr_j.t[0:64], QR[j], reads=[bQ[j], qr_j.b], writes=[qr_j.b2])
                ps = PS[n % 2]
                with s.grp("pe", [bk[kt], bkN[kt], KRs.b, qn_j.b, qr_j.b, qr_j.b2], [ps[0].b, ps[1].b]) as g:
                    for h in range(8):
                        nc.tensor.matmul(ps[h // 4].t[:, h % 4, :], lhsT=KNs.t[:, kt, h, :], rhs=qn_j.t[:, h, :], start=True, stop=False)
                        g.last = nc.tensor.matmul(ps[h // 4].t[:, h % 4, :], lhsT=KRs.t[:, kt, :], rhs=qr_j.t[:, h, :], start=False, stop=True)

            if pairs:
                emit_S(0)
            for n, (j, kt) in enumerate(pairs):
                io = 2 * j + 1
                ps, pt_ = PS[n % 2], pT[n % 2]
                if n + 1 < len(pairs):
                    emit_S(n + 1)
                for hb in range(2):
                    if kt == 0:
                        s.op("act", lambda: A.activation(out=pt_.t[:, hb * 4:hb * 4 + 4, :], in_=ps[hb].t[:], func=AF.Exp, scale=scale,
                                                         bias=vecs.t[:, C_NEG:C_NEG + 1]), [ps[hb].b, vecs.b], [pt_.b])
                    else:
                        s.op("act", lambda: A.activation(out=pt_.t[:, hb * 4:hb * 4 + 4, :], in_=ps[hb].t[:], func=AF.Exp, scale=scale),
                             [ps[hb].b], [pt_.b])
                if kt == io:
                    s.op("pool", lambda: P.tensor_tensor(out=pt_.t[:], in0=pt_.t[:],
                                                         in1=trib.t[:, None, :].to_broadcast([128, 8, 128]), op=ALU.mult),
                         [pt_.b, trib.b], [pt_.b])
                if kt % 2 == 0:
                    if kt == 0:
                        s.op("pool", lambda: P.tensor_copy(out=Lacc.t[:], in_=pt_.t[:]), [pt_.b], [Lacc.b])
                    else:
                        s.op("pool", lambda: P.tensor_tensor(out=Lacc.t[:], in0=Lacc.t[:], in1=pt_.t[:], op=ALU.add),
                             [Lacc.b, pt_.b], [Lacc.b])
                else:
                    if kt == 1:
                        s.op("dve", lambda: V.tensor_copy(out=LaccD.t[:], in_=pt_.t[:]), [pt_.b], [LaccD.b])
                    else:
                        s.op("dve", lambda: V.tensor_tensor(out=LaccD.t[:], in0=LaccD.t[:], in1=pt_.t[:], op=ALU.add),
                             [LaccD.b, pt_.b], [LaccD.b])
                with s.grp("pe", [bkV[kt], pt_.b], [PO[0].b, PO[1].b]) as g:
                    for h in range(8):
                        st_ = (kt == 0 and h % 4 == 0)
                        g.last = nc.tensor.matmul(PO[h // 4].t[:, h % 4, :], lhsT=VSs.t[:, kt, h, :], rhs=pt_.t[:, h, :],
                                                  start=st_, stop=(kt == io and h % 4 == 3), skip_group_check=True)
                if kt != io:
                    continue
                with s.grp("pe", [Lacc.b, LaccD.b, onesf.b], [PL[0].b, PL[1].b]) as g:
                    for hb in range(2):
                        nc.tensor.matmul(fl(PL[hb]), lhsT=onesf.t[:], rhs=Lacc.t[:, hb * 4:hb * 4 + 4, :].rearrange("p a b -> p (a b)"),
                                         start=True, stop=False)
                        g.last = nc.tensor.matmul(fl(PL[hb]), lhsT=onesf.t[:], rhs=LaccD.t[:, hb * 4:hb * 4 + 4, :].rearrange("p a b -> p (a b)"),
                                                  start=False, stop=True)
                for hb in range(2):
                    hs = slice(hb * 4, hb * 4 + 4)
                    s.op("dve", lambda: V.reciprocal(out=Lr.t[:, hs, :], in_=PL[hb].t[:]), [PL[hb].b], [Lr.b])
                    s.op("dve", lambda: V.tensor_tensor(out=of.t[:, hs, :], in0=PO[hb].t[:], in1=Lr.t[:, hs, :], op=ALU.mult),
                         [PO[hb].b, Lr.b], [of.b])
                s.op("act", lambda: A.activation(out=osq.t[:], in_=of.t[:], func=AF.Square), [of.b], [osq.b])
                with s.grp("pe", [osq.b, onesb.b], [PL[0].b, PL[1].b]) as g:
                    for h in range(8):
                        g.last = nc.tensor.matmul(PL[h // 4].t[:, h % 4, :], lhsT=onesb.t[:], rhs=osq.t[:, h, :], start=True, stop=True)
                for hb in range(2):
                    rstd_from(None, PL[hb].t[:], 128, [PL[hb].b], rs8, rs8.t[:, hb * 4:hb * 4 + 4, :])
                s.op("dve", lambda: V.tensor_tensor(out=of.t[:], in0=of.t[:], in1=rs8.t[:], op=ALU.mult), [of.b, rs8.b], [of.b])
                s.op("pool", lambda: P.tensor_tensor(out=mixm.t[:], in0=of.t[:],
                                                     in1=vecs.t[:, C_MOG:C_MOG + 8, None].to_broadcast([128, 8, 128]), op=ALU.mult),
                     [of.b, vecs.b], [mixm.b])
                s.dma("sp", MIX[j, :, 0:8, :], mixm.t[:], reads=[mixm.b], writes=[bMIXm[j]])


        s.barrier()
        with ExitStack() as p3:
            skf = sbt(p3, "skf", [128, 16, 128], F32)
            skb = sbt(p3, "skb", [128, 16, 128], BF16)
            PT = pst(p3, "PT3s", [128, 16, 128], BF16)
            s.dma("sp", skf.t[:], subk.rearrange("c k d -> k c d"), writes=[skf.b])
            s.op("dve", lambda: V.tensor_copy(out=skb.t[:], in_=skf.t[:]), [skf.b], [skb.b])
            with s.grp("pe", [skb.b, identb.b], [PT.b]) as g:
                for c in range(16):
                    g.last = nc.tensor.transpose(PT.t[:, c, :], skb.t[:, c, :], identb.t[:])
            s.op("dve", lambda: V.tensor_copy(out=skT.t[:], in_=PT.t[:]), [PT.b], [skT.b])

        s.barrier()
        with ExitStack() as p3:
            wo = sbt(p3, "wo", [128, 16, D], BF16)
            wgr = sbt(p3, "wgr", [128, 16, 1024], BF16)
            hb1 = [sbt(p3, "hb1%d" % i, [128, 16, 128], BF16) for i in range(2)]
            sg = sbt(p3, "sg", [128, 8, 128], BF16)
            g1b = sbt(p3, "g1b", [128, D], F32)
            w_out_v = w_out.rearrange("(k p) n -> p k n", p=128)
            w_in_v = w_in.rearrange("(k p) n -> p k n", p=128)
            wgrb = [Buf() for _ in range(2)]
            wob = [Buf() for _ in range(4)]
            for hb in range(2):
                s.dma("pool", wgr.t[:, :, hb * 512:(hb + 1) * 512], w_in_v[:, :, O_GR + hb * 512:O_GR + (hb + 1) * 512], writes=[wgrb[hb]])
            for blk in range(4):
                s.dma("pool", wo.t[:, :, blk * 512:(blk + 1) * 512], w_out_v[:, :, blk * 512:(blk + 1) * 512], writes=[wob[blk]])
            s.dma("sp", g1b.t[:], G12[0], reads=[bG12], writes=[g1b.b])
            PT = pst(p3, "PT3", [128, 16, 128], BF16)
            PB = [pst(p3, "PB3%d" % i, [128, 4, 128]) for i in range(6)]
            pbi = [0]

            def bank3():
                b = PB[pbi[0] % 6]
                pbi[0] += 1
                return b
            mx = [sbt(p3, "mx%d" % i, [128, 16, 128], BF16) for i in range(2)]
            x3 = [sbt(p3, "x3%d" % i, [128, D], F32) for i in range(2)]
            x2 = [sbt(p3, "x2t%d" % i, [128, D], F32) for i in range(2)]
            junk = sbt(p3, "junk3", [128, D], BF16)
            xn = sbt(p3, "xn3", [128, D], BF16)
            h2 = [sbt(p3, "h2%d" % i, [128, 16, 128], BF16) for i in range(2)]
            st1 = sbt(p3, "st13", [128, 4], F32)

            def load3(j):
                s.dma("sp", hb1[j % 2].t[:], HT1[j], reads=[bHT1[j]], writes=[hb1[j % 2].b])
                s.dma("sp", mx[j % 2].t[:], MIX[j], reads=[bMIXm[j], bMIXg[j]], writes=[mx[j % 2].b])
                s.dma("sp", x3[j % 2].t[:], xs[(2 * j + 1) * 128:(2 * j + 2) * 128, :], writes=[x3[j % 2].b])
            load3(0)
            for j in range(NOWN if stop >= 4 else 0):
                mxj, xj, x2j, h2j = mx[j % 2], x3[j % 2], x2[j % 2], h2[j % 2]
                hj = hb1[j % 2]
                if j + 1 < NOWN:
                    load3(j + 1)
                for hb in range(2):
                    pgr = bank3()
                    with s.grp("pe", [hj.b, wgrb[hb]], [pgr.b]) as g:
                        for c in range(4):
                            col0 = (hb * 4 + c) * 128
                            for k in range(16):
                                g.last = nc.tensor.matmul(pgr.t[:, c, :], lhsT=wgr.t[:, k, col0:col0 + 128], rhs=hj.t[:, k, :],
                                                          start=(k == 0), stop=(k == 15))
                    s.op("act", lambda: A.activation(out=sg.t[:, hb * 4:hb * 4 + 4, :], in_=pgr.t[:], func=AF.Silu), [pgr.b], [sg.b])
                s.op("dve", lambda: V.tensor_tensor(out=mxj.t[:, 8:16, :], in0=mxj.t[:, 8:16, :], in1=sg.t[:], op=ALU.mult),
                     [mxj.b, sg.b], [mxj.b])
                for blk in range(4):
                    pb = bank3()
                    with s.grp("pe", [mxj.b, wob[blk]], [pb.b]) as g:
                        for k in range(16):
                            g.last = nc.tensor.matmul(fl(pb), lhsT=mxj.t[:, k, :], rhs=wo.t[:, k, blk * 512:(blk + 1) * 512],
                                                      start=(k == 0), stop=(k == 15))
                    bs = slice(blk * 512, (blk + 1) * 512)
                    s.op("dve", lambda: V.tensor_tensor(out=x2j.t[:, bs], in0=fl(pb), in1=g1b.t[:, bs], op=ALU.mult),
                         [pb.b, g1b.b], [x2j.b])
                s.op("pool", lambda: P.tensor_tensor(out=x2j.t[:], in0=x2j.t[:], in1=xj.t[:], op=ALU.add),
                     [x2j.b, xj.b], [x2j.b])
                s.dma("sp", X2[j], x2j.t[:], reads=[x2j.b], writes=[bX2[j]])
                s.op("act", lambda: A.activation(out=junk.t[:], in_=x2j.t[:], func=AF.Square, accum_out=st1.t[:, 0:1]),
                     [x2j.b], [junk.b, st1.b])
                rstd_from(None, st1.t[:, 0:1], D, [st1.b], st1, st1.t[:, 1:2])
                s.op("act", lambda: A.activation(out=xn.t[:], in_=x2j.t[:], func=AF.Copy, scale=st1.t[:, 1:2]),
                     [x2j.b, st1.b], [xn.b])
                with s.grp("pe", [xn.b, identb.b], [PT.b]) as g:
                    for k in range(16):
                        g.last = nc.tensor.transpose(PT.t[:, k, :], xn.t[:, k * 128:(k + 1) * 128], identb.t[:])
                for k in range(16):
                    if k % 2 == 0:
                        s.op("dve", lambda: V.tensor_scalar(out=h2j.t[:, k, :], in0=PT.t[:, k, :], scalar1=AB.t[:, 32 + k:33 + k],
                                                            scalar2=AB.t[:, 48 + k:49 + k], op0=ALU.mult, op1=ALU.add),
                             [PT.b, AB.b], [h2j.b])
                    else:
                        s.op("act", lambda: A.activation(out=h2j.t[:, k, :], in_=PT.t[:, k, :], func=AF.Identity,
                                                         scale=AB.t[:, 32 + k:33 + k], bias=AB.t[:, 48 + k:49 + k]),
                             [PT.b, AB.b], [h2j.b2])
                s.dma("sp", H2T[j], h2j.t[:], reads=[h2j.b, h2j.b2], writes=[bH2T[j]])

        s.barrier()
        with ExitStack() as p3:
            U32 = mybir.dt.uint32
            wq = sbt(p3, "wq", [128, 16, D], BF16)
            w_pq_v = w_pq.rearrange("(k p) n -> p k n", p=128)
            wqb = [Buf() for _ in range(4)]
            for cb in range(4):
                s.dma("pool", wq.t[:, :, cb * 512:(cb + 1) * 512], w_pq_v[:, :, cb * 512:(cb + 1) * 512], writes=[wqb[cb]])
            PB = [pst(p3, "PB4%d" % i, [128, 4, 128]) for i in range(6)]
            PT = pst(p3, "PT4", [128, 8, 128], BF16)
            pbi = [0]

            def bank3():
                b = PB[pbi[0] % 6]
                pbi[0] += 1
                return b
            h2 = [sbt(p3, "h2b%d" % i, [128, 16, 128], BF16) for i in range(2)]
            qpT = sbt(p3, "qpT", [128, 16, 128], BF16)
            sall = [sbt(p3, "sall%d" % i, [128, 16, 128], F32) for i in range(2)]
            wk = sbt(p3, "wk", [128, 256], F32)
            a16 = sbt(p3, "a16", [128, 16, 16], F32)
            i16u = sbt(p3, "i16u", [128, 16, 16], U32)
            i16 = sbt(p3, "i16", [128, 16, 16], F32)
            cand = sbt(p3, "cand", [128, 8, 256], F32)
            best = sbt(p3, "best", [128, 8, 16], F32)
            bixu = sbt(p3, "bixu", [128, 8, 16], U32)
            bix = sbt(p3, "bix", [128, 8, 16], F32)
            mm = sbt(p3, "mm", [128, 16], F32)
            eb = sbt(p3, "eb", [128, 8, 16], F32)
            u17 = sbt(p3, "u17", [128, 8, 16, 17], F32)
            sel = sbt(p3, "sel", [128, 8, 16, 16], F32)
            jv = sbt(p3, "jv", [128, 8, 16], F32)
            kv_ = sbt(p3, "kv_", [128, 8, 16], F32)
            lists = sbt(p3, "lists", [128, 3, 128], F32)
            listsb = sbt(p3, "listsb", [128, 3, 128], BF16)
            pmj = [sbt(p3, "pmaj%d" % i, [128, 3, 128], BF16) for i in range(2)]
            io17 = sbt(p3, "io17", [128, 17], F32)
            io128 = sbt(p3, "io128", [128, 128], F32)
            io128b = sbt(p3, "io128b", [128, 128], BF16)
            TH = 32
            Rms = [sbt(p3, "Rm%d" % i, [128, TH, 128], BF16) for i in range(2)]
            OHs = [sbt(p3, "OH%d" % i, [128, TH, 128], BF16) for i in range(2)]
            wTt = sbt(p3, "wTt", [128, 128, 128], BF16)
            io128i = sbt(p3, "io128i", [128, 128], I32)
            s.op("pool", lambda: P.iota(io128i.t[:], pattern=[[1, 128]], base=0, channel_multiplier=0), [], [io128i.b])
            s.op("dve", lambda: V.tensor_copy(out=io128.t[:], in_=io128i.t[:]), [io128i.b], [io128.b])
            s.op("dve", lambda: V.tensor_copy(out=io128b.t[:], in_=io128.t[:]), [io128.b], [io128b.b])
            s.op("dve", lambda: V.tensor_scalar(out=io17.t[:], in0=io128.t[:, 0:17], scalar1=16.0, scalar2=None, op0=ALU.mult),
                 [io128.b], [io17.b])
            s.dma("sp", h2[0].t[:], H2T[0], reads=[bH2T[0]], writes=[h2[0].b])
            def stageQ(j):
                h2j, sal_ = h2[j % 2], sall[j % 2]
                if j + 1 < NOWN:
                    s.dma("sp", h2[(j + 1) % 2].t[:], H2T[j + 1], reads=[bH2T[j + 1]], writes=[h2[(j + 1) % 2].b])
                for cb in range(4):
                    pb = bank3()
                    with s.grp("pe", [h2j.b, wqb[cb]], [pb.b]) as g:
                        for cc in range(4):
                            c = cb * 4 + cc
                            for k in range(16):
                                g.last = nc.tensor.matmul(pb.t[:, cc, :], lhsT=wq.t[:, k, c * 128:(c + 1) * 128], rhs=h2j.t[:, k, :],
                                                          start=(k == 0), stop=(k == 15))
                    s.op("act", lambda: A.copy(out=qpT.t[:, cb * 4:cb * 4 + 4, :], in_=pb.t[:]), [pb.b], [qpT.b])
                for cb in range(4):
                    pb = bank3()
                    with s.grp("pe", [qpT.b, skT.b], [pb.b]) as g:
                        for cc in range(4):
                            c = cb * 4 + cc
                            g.last = nc.tensor.matmul(pb.t[:, cc, :], lhsT=qpT.t[:, c, :], rhs=skT.t[:, c, :], start=True, stop=True)
                    s.op("act", lambda: A.copy(out=sal_.t[:, cb * 4:cb * 4 + 4, :], in_=pb.t[:]), [pb.b], [sal_.b])
            def stageT(j):
                sal_, pmaj = sall[j % 2], pmj[j % 2]
                for c in range(16):
                    s.op("dve", lambda: V.max(out=a16.t[:, c, 0:8], in_=sal_.t[:, c, :]), [sal_.b], [a16.b])
                    s.op("dve", lambda: V.max_index(out=i16u.t[:, c, 0:8], in_max=a16.t[:, c, 0:8], in_values=sal_.t[:, c, :]),
                         [sal_.b, a16.b], [i16u.b])
                    s.op("dve", lambda: V.match_replace(out=wk.t[:, 0:128], in_to_replace=a16.t[:, c, 0:8], in_values=sal_.t[:, c, :],
                                                        imm_value=-1e30), [sal_.b, a16.b], [wk.b])
                    s.op("dve", lambda: V.max(out=a16.t[:, c, 8:16], in_=wk.t[:, 0:128]), [wk.b], [a16.b])
                    s.op("dve", lambda: V.max_index(out=i16u.t[:, c, 8:16], in_max=a16.t[:, c, 8:16], in_values=wk.t[:, 0:128]),
                         [wk.b, a16.b], [i16u.b])
                s.op("dve", lambda: V.tensor_copy(out=i16.t[:], in_=i16u.t[:]), [i16u.b], [i16.b])
                a4 = a16.t[:].rearrange("p (h two) k -> p h two k", two=2)
                i4 = i16.t[:].rearrange("p (h two) k -> p h two k", two=2)
                c4 = cand.t[:].rearrange("p h (a b) -> p h a b", b=16)
                s.op("dve", lambda: V.tensor_tensor(out=c4, in0=a4[:, :, 0, :, None].to_broadcast([128, 8, 16, 16]),
                                                    in1=a4[:, :, 1, None, :].to_broadcast([128, 8, 16, 16]), op=ALU.add),
                     [a16.b], [cand.b])
                for h in range(8):
                    s.op("dve", lambda: V.max(out=best.t[:, h, 0:8], in_=cand.t[:, h, :]), [cand.b], [best.b])
                    s.op("dve", lambda: V.max_index(out=bixu.t[:, h, 0:8], in_max=best.t[:, h, 0:8], in_values=cand.t[:, h, :]),
                         [cand.b, best.b], [bixu.b])
                    s.op("dve", lambda: V.match_replace(out=wk.t[:], in_to_replace=best.t[:, h, 0:8], in_values=cand.t[:, h, :],
                                                        imm_value=-1e30), [cand.b, best.b], [wk.b])
                    s.op("dve", lambda: V.max(out=best.t[:, h, 8:16], in_=wk.t[:]), [wk.b], [best.b])
                    s.op("dve", lambda: V.max_index(out=bixu.t[:, h, 8:16], in_max=best.t[:, h, 8:16], in_values=wk.t[:]),
                         [wk.b, best.b], [bixu.b])
                s.op("dve", lambda: V.tensor_copy(out=bix.t[:], in_=bixu.t[:]), [bixu.b], [bix.b])
                s.op("dve", lambda: V.tensor_copy(out=mm.t[:, 0:8], in_=best.t[:, :, 0]), [best.b], [mm.b])
                s.op("dve", lambda: V.tensor_tensor(out=eb.t[:], in0=best.t[:], in1=mm.t[:, 0:8, None].to_broadcast([128, 8, 16]),
                                                    op=ALU.subtract), [best.b, mm.b], [eb.b])
                s.op("act", lambda: A.activation(out=eb.t[:], in_=eb.t[:], func=AF.Exp), [eb.b], [eb.b])
                s.op("dve", lambda: V.reduce_sum(out=mm.t[:, 8:16], in_=eb.t[:], axis=AX.X), [eb.b], [mm.b])
                s.op("dve", lambda: V.reciprocal(out=mm.t[:, 8:16], in_=mm.t[:, 8:16]), [mm.b], [mm.b])
                l3 = lists.t[:].rearrange("p a (h r) -> p a h r", r=16)
                s.op("dve", lambda: V.tensor_tensor(out=l3[:, 2], in0=eb.t[:], in1=mm.t[:, 8:16, None].to_broadcast([128, 8, 16]),
                                                    op=ALU.mult), [eb.b, mm.b], [lists.b])
                s.op("dve", lambda: V.tensor_tensor(out=u17.t[:], in0=bix.t[:, :, :, None].to_broadcast([128, 8, 16, 17]),
                                                    in1=io17.t[:, None, None, :].to_broadcast([128, 8, 16, 17]), op=ALU.is_ge),
                     [bix.b, io17.b], [u17.b])
                s.op("dve", lambda: V.tensor_tensor(out=sel.t[:], in0=u17.t[:, :, :, 0:16], in1=u17.t[:, :, :, 1:17], op=ALU.subtract),
                     [u17.b], [sel.b])
                s.op("dve", lambda: V.tensor_tensor(out=u17.t[:, :, :, 0:16], in0=sel.t[:], in1=i4[:, :, 0, None, :].to_broadcast([128, 8, 16, 16]),
                                                    op=ALU.mult), [sel.b, i16.b], [u17.b])
                s.op("dve", lambda: V.reduce_sum(out=l3[:, 0], in_=u17.t[:, :, :, 0:16], axis=AX.X), [u17.b], [lists.b])
                s.op("dve", lambda: V.tensor_tensor(out=u17.t[:, :, :, 0:16], in0=sel.t[:],
                                                    in1=io128.t[:, None, None, 0:16].to_broadcast([128, 8, 16, 16]), op=ALU.mult),
                     [sel.b, io128.b, u17.b], [u17.b])
                s.op("dve", lambda: V.reduce_sum(out=jv.t[:], in_=u17.t[:, :, :, 0:16], axis=AX.X), [u17.b], [jv.b])
                s.op("dve", lambda: V.scalar_tensor_tensor(out=kv_.t[:], in0=jv.t[:], scalar=-16.0, in1=bix.t[:], op0=ALU.mult, op1=ALU.add),
                     [jv.b, bix.b], [kv_.b])
                s.op("dve", lambda: V.tensor_tensor(out=sel.t[:], in0=kv_.t[:, :, :, None].to_broadcast([128, 8, 16, 16]),
                                                    in1=io128.t[:, None, None, 0:16].to_broadcast([128, 8, 16, 16]), op=ALU.is_equal),
                     [kv_.b, io128.b, sel.b], [sel.b])
                s.op("dve", lambda: V.tensor_tensor(out=u17.t[:, :, :, 0:16], in0=sel.t[:], in1=i4[:, :, 1, None, :].to_broadcast([128, 8, 16, 16]),
                                                    op=ALU.mult), [sel.b, i16.b, u17.b], [u17.b])
                s.op("dve", lambda: V.reduce_sum(out=l3[:, 1], in_=u17.t[:, :, :, 0:16], axis=AX.X), [u17.b], [lists.b])
                s.op("dve", lambda: V.tensor_copy(out=listsb.t[:], in_=lists.t[:]), [lists.b], [listsb.b])
                with s.grp("pe", [listsb.b, identb.b], [PT.b]) as g:
                    for a_ in range(3):
                        g.last = nc.tensor.transpose(PT.t[:, a_, :], listsb.t[:, a_, :], identb.t[:])
                s.op("act", lambda: A.copy(out=pmaj.t[:], in_=PT.t[:, 0:3, :]), [PT.b], [pmaj.b])
            def stageE(j):
                pmaj = pmj[j % 2]
                for half in range(128 // TH):
                    Rm, OH = Rms[half % 2], OHs[half % 2]
                    ts_ = slice(half * TH, (half + 1) * TH)
                    s.op("dve", lambda: V.tensor_tensor(out=Rm.t[:], in0=io128b.t[:, None, :].to_broadcast([128, TH, 128]),
                                                        in1=pmaj.t[:, 1, ts_, None].to_broadcast([128, TH, 128]), op=ALU.is_equal),
                         [io128b.b, pmaj.b], [Rm.b])
                    s.op("pool", lambda: P.tensor_tensor(out=Rm.t[:], in0=Rm.t[:],
                                                         in1=pmaj.t[:, 2, ts_, None].to_broadcast([128, TH, 128]), op=ALU.mult),
                         [Rm.b, pmaj.b], [Rm.b])
                    s.op("dve", lambda: V.tensor_tensor(out=OH.t[:], in0=io128b.t[:, None, :].to_broadcast([128, TH, 128]),
                                                        in1=pmaj.t[:, 0, ts_, None].to_broadcast([128, TH, 128]), op=ALU.is_equal),
                         [io128b.b, pmaj.b], [OH.b])
                    for q4 in range(TH // 4):
                        pb = bank3()
                        with s.grp("pe", [Rm.b, OH.b], [pb.b]) as g:
                            for tl in range(4):
                                t_ = q4 * 4 + tl
                                g.last = nc.tensor.matmul(pb.t[:, tl, :], lhsT=Rm.t[:, t_, :], rhs=OH.t[:, t_, :], start=True, stop=True)
                        t0 = half * TH + q4 * 4
                        dst = wTt.t[:, :, t0:t0 + 4]
                        src = pb.t[:].rearrange("p t i -> p i t")
                        s.op("act", lambda: A.copy(out=dst, in_=src), [pb.b], [wTt.b])
                s.dma("sp", WT[j], wTt.t[:], reads=[wTt.b], writes=[bSALL[j]])
            if stop >= 5:
                stageQ(0)
                stageT(0)
                for j in range(NOWN):
                    if j + 1 < NOWN:
                        stageQ(j + 1)
                    stageE(j)
                    if j + 1 < NOWN:
                        stageT(j + 1)

        NSUP = 16
        NG = 4
        s.barrier()
        with ExitStack() as p4:
            g2b = sbt(p4, "g2b", [128, D], F32)
            s.dma("sp", g2b.t[:], G12[1], reads=[bG12], writes=[g2b.b])
            ubc = [sbt(p4, "ubc%d" % i, [128, D], BF16) for i in range(2)]
            uT = sbt(p4, "uT", [128, 16, 1024], BF16)
            vb = [sbt(p4, "vb%d" % i, [128, 8, D], BF16) for i in range(2)]
            hg = [sbt(p4, "hg%d" % i, [128, 16, 512], BF16) for i in range(2)]
            wtg = [sbt(p4, "wtg%d" % i, [128, 4, 8, 128], BF16) for i in range(2)]
            gT_ = [sbt(p4, "gT%d" % i, [128, 512], BF16) for i in range(2)]
            zT = [sbt(p4, "zT%d" % i, [128, 8, 512], BF16) for i in range(2)]
            outps = [sbt(p4, "outp%d" % i, [128, D], F32) for i in range(2)]
            PUs = [pst(p4, "PU%d" % i, [128, 8, 128], BF16) for i in range(2)]
            PA = [pst(p4, "PA%d" % i, [128, 512]) for i in range(2)]
            PO = [pst(p4, "PO4%d" % i, [128, 512]) for i in range(4)]
            uview = u_tab.rearrange("(s c p) d -> s p c d", p=128, c=8)
            vview = v_tab.rearrange("(s c p) d -> s p c d", p=128, c=8)

            def load_u(sc_, c_):
                s.dma("pool", ubc[c_ % 2].t[:], uview[sc_, :, c_, :], writes=[ubc[c_ % 2].b])

            def load_v(sc_):
                for c_ in range(8):
                    s.dma("pool", vb[sc_ % 2].t[:, c_, :], vview[sc_, :, c_, :], writes=[vb[sc_ % 2].b])

            def load_group(sc_, g_, sl):
                for tl in range(4):
                    tt = g_ * 4 + tl
                    s.dma("sp", hg[sl].t[:, :, tl * 128:(tl + 1) * 128], H2T[tt], reads=[bH2T[tt]], writes=[hg[sl].b])
                    s.dma("sp", wtg[sl].t[:, tl], WT[tt, :, sc_ * 8:sc_ * 8 + 8, :], reads=[bSALL[tt]], writes=[wtg[sl].b])

            for c_ in range(2):
                load_u(0, c_)
            load_v(0)
            it = 0
            if stop >= 6:
                load_group(0, 0, 0)
            for sc in range(NSUP if stop >= 6 else 0):
                vbs = vb[sc % 2]
                for c in range(8):
                    ub_ = ubc[c % 2]
                    for kk in range(2):
                        PU = PUs[kk]
                        with s.grp("pe", [ub_.b, identb.b], [PU.b]) as g:
                            for k8 in range(8):
                                k = kk * 8 + k8
                                g.last = nc.tensor.transpose(PU.t[:, k8, :], ub_.t[:, k * 128:(k + 1) * 128], identb.t[:])
                        dst = uT.t[:, kk * 8:kk * 8 + 8, c * 128:(c + 1) * 128]
                        if (c * 2 + kk) % 2 == 0:
                            s.op("act", lambda: A.copy(out=dst, in_=PU.t[:]), [PU.b], [uT.b2])
                        else:
                            s.op("dve", lambda: V.tensor_copy(out=dst, in_=PU.t[:]), [PU.b], [uT.b])
                    if c + 2 < 8:
                        load_u(sc, c + 2)
                    elif sc + 1 < NSUP:
                        load_u(sc + 1, c - 6)
                if sc + 1 < NSUP:
                    load_v(sc + 1)
                for g_ in range(NG):
                    sl = it % 2
                    it += 1
                    if g_ + 1 < NG:
                        load_group(sc, g_ + 1, 1 - sl)
                    elif sc + 1 < NSUP:
                        load_group(sc + 1, 0, 1 - sl)
                    hgs, wts, zTs = hg[sl], wtg[sl], zT[sl]
                    for c in range(8):
                        pa = PA[c % 2]
                        with s.grp("pe", [hgs.b, uT.b, uT.b2], [pa.b]) as g:
                            for k in range(16):
                                g.last = nc.tensor.matmul(pa.t[:], lhsT=uT.t[:, k, c * 128:(c + 1) * 128], rhs=hgs.t[:, k, :],
                                                          start=(k == 0), stop=(k == 15))
                        gt = gT_[c % 2]
                        s.op("act", lambda: A.activation(out=gt.t[:], in_=pa.t[:], func=AF.Gelu), [pa.b], [gt.b])
                        s.op("dve", lambda: V.tensor_tensor(out=zTs.t[:, c, :].rearrange("p (a b) -> p a b", a=4), in0=gt.t[:].rearrange("p (a b) -> p a b", a=4),
                                                            in1=wts.t[:, :, c, :], op=ALU.mult), [gt.b, wts.b], [zTs.b])
                    for tl in range(4):
                        tt = g_ * 4 + tl
                        outp = outps[tl % 2]
                        for blk in range(4):
                            with s.grp("pe", [zTs.b, vbs.b], [PO[blk].b]) as g:
                                for c in range(8):
                                    g.last = nc.tensor.matmul(PO[blk].t[:], lhsT=zTs.t[:, c, tl * 128:(tl + 1) * 128],
                                                              rhs=vbs.t[:, c, blk * 512:(blk + 1) * 512], start=(c == 0), stop=(c == 7))
                            osl = slice(blk * 512, (blk + 1) * 512)
                            s.op("dve", lambda: V.tensor_tensor(out=outp.t[:, osl], in0=PO[blk].t[:], in1=g2b.t[:, osl], op=ALU.mult),
                                 [PO[blk].b, g2b.b], [outp.b])
                        s.dma("pool", X2[tt], outp.t[:], reads=[outp.b, outp.b2], writes=[bX2[tt]], accum_op=ALU.add)

        s.barrier()
        with ExitStack() as p5:
            fg = sbt(p5, "fg", [128, D], F32)
            s.dma("sp", fg.t[:], fin_g.to_broadcast([128, D]), writes=[fg.b])
            xf = [sbt(p5, "xf%d" % i, [128, D], F32) for i in range(2)]
            yo = [sbt(p5, "yo%d" % i, [128, D], F32) for i in range(2)]
            junk = sbt(p5, "junk5", [128, D], BF16)
            st = [sbt(p5, "st5%d" % i, [128, 2], F32) for i in range(2)]
            for tt in range(NOWN if stop >= 7 else 0):
                xx, yy, ss = xf[tt % 2], yo[tt % 2], st[tt % 2]
                s.dma("sp", xx.t[:], X2[tt], reads=[bX2[tt]], writes=[xx.b])
                s.op("act", lambda: A.activation(out=junk.t[:], in_=xx.t[:], func=AF.Square, accum_out=ss.t[:, 0:1]),
                     [xx.b], [junk.b, ss.b])
                rstd_from(None, ss.t[:, 0:1], D, [ss.b], ss, ss.t[:, 1:2])
                s.op("act", lambda: A.activation(out=yy.t[:], in_=xx.t[:], func=AF.Copy, scale=ss.t[:, 1:2]), [xx.b, ss.b], [yy.b])
                s.op("dve", lambda: V.tensor_tensor(out=yy.t[:], in0=yy.t[:], in1=fg.t[:], op=ALU.mult), [yy.b, fg.b], [yy.b])
                s.dma("sp", y_out[tt * 128:(tt + 1) * 128, :], yy.t[:], reads=[yy.b], writes=[bY[tt]])
            s.finish("sp", bY)
            if debug:
                s.finish("sp", [bROPE, bG12] + bKN + bKR + bVS + bQ + bMIXm + bMIXg + bX2 + bH2T + bSALL + bHT1)
    return nc


_NC_CACHE = {}


def _consts():
    j = np.arange(128)
    T_ = (j[:, None] <= j[None, :]).astype(np.float32)
    E_ = np.broadcast_to((j[:, None] <= 63).astype(np.float32), (128, 128))
    cm = np.stack([np.eye(128, dtype=np.float32), T_, T_ - E_, T_, 1.0 - T_], axis=1)
    return np.ascontiguousarray(cm.astype(np.float32))


def kernel(x, c, positions, ada_w, ada_b, mix_norm_g, w_in, mla_q_norm_g, mla_w_uq,
           mla_kv_norm_g, mla_w_ukv, mla_out_norm_g, gla_w_gate2, gla_b_gate,
           gla_out_norm_g, w_out, ffn_norm_g, peer_w_q, peer_sub_keys, peer_u, peer_v,
           final_norm_g):
    f = lambda a: np.ascontiguousarray(np.asarray(a, dtype=np.float32))
    x = f(x)
    positions = np.asarray(positions).astype(np.int32)
    B, S, _ = x.shape
    if "nc" not in _NC_CACHE:
        _NC_CACHE["nc"] = build_program()
    nc = _NC_CACHE["nc"]
    cm = _consts()
    col = lambda v, n: f(v).reshape(n, 128).T
    invf = (10000.0 ** (-np.arange(0, 64, 2, dtype=np.float32) / 64.0)).astype(np.float32)
    shared = {
        "cmat": cm, "ada_w": f(ada_w[0]), "ada_b": f(ada_b[0]).reshape(1, -1), "w_in": f(w_in[0]),
        "w_uq": f(mla_w_uq[0]), "w_ukv": f(mla_w_ukv[0]), "w_g2": f(gla_w_gate2[0]), "b_g": f(gla_b_gate[0]).reshape(1, -1),
        "w_out": f(w_out[0]), "w_pq": f(peer_w_q[0]), "subk": f(peer_sub_keys[0]).reshape(16, 128, 128),
        "u_tab": f(peer_u[0]), "v_tab": f(peer_v[0]), "fin_g": f(final_norm_g).reshape(1, -1),
    }
    in_maps = []
    for core in range(8):
        b, p = core // 2, core % 2
        xt = x[b].reshape(32, 128, D)
        pt = positions[b].reshape(32, 128)
        if p == 1:
            xs, ps = xt, pt
        else:
            xs = np.concatenate([np.zeros((1, 128, D), np.float32), xt[:31]], axis=0)
            ps = np.concatenate([np.zeros((1, 128), np.int32), pt[:31]], axis=0)
        vec = np.zeros((128, NVEC), np.float32)
        vec[:, C_C:C_C + 16] = col(c[b], 16)
        vec[:, C_G1:C_G1 + 16] = col(mix_norm_g[0], 16)
        vec[:, C_G2:C_G2 + 16] = col(ffn_norm_g[0], 16)
        vec[:, C_QG:C_QG + 4] = col(mla_q_norm_g[0], 4)
        vec[:, C_KVG:C_KVG + 2] = col(mla_kv_norm_g[0], 2)
        vec[:, C_MOG:C_MOG + 8] = col(mla_out_norm_g[0], 8)
        vec[:, C_GOG:C_GOG + 8] = col(gla_out_norm_g[0], 8)
        vec[0:64, C_INVF] = np.concatenate([invf, invf]) / np.float32(2 * np.pi)
        vec[0:32, C_SGN] = -1.0
        vec[32:64, C_SGN] = 1.0
        vec[:, C_FLAG] = float(p)
        vec[:, C_NEG] = (float(p) - 1.0) * 30000.0
        vec[:, C_ONE] = 1.0
        m = dict(shared)
        m["xs"] = np.ascontiguousarray(xs.reshape(NT * 128, D))
        m["pos"] = np.ascontiguousarray(ps.reshape(1, NT * 128))
        m["vec"] = vec
        in_maps.append(m)
    res = run_bass_kernel_spmd(nc, in_maps, core_ids=list(range(8)))
    out = np.empty((B, 32, 128, D), np.float32)
    for core in range(8):
        b, p = core // 2, core % 2
        y = np.asarray(res.results[core]["y_out"]).reshape(16, 128, D)
        out[b, p::2] = y
    return out.reshape(B, S, D)
```
